# Optimizing a Trainium2 kernel written in Bass

```python
import math
import jax, jax.numpy as jnp
from jax import lax
import numpy as np

D_MODEL = 1024
BATCH = 16
SEQ = 4096
DEPTH = 4

GRID_W = 64
CTX_LEN = 256
D_MIX = D_MODEL
NA_HEADS = 8
NA_HEAD_DIM = 64
NA_WIDTH = NA_HEADS * NA_HEAD_DIM
NA_KR = 8
NA_KC = 16
DN_HEAD_DIM = 128
DN_WIDTH = D_MIX - NA_WIDTH
DN_HEADS = DN_WIDTH // DN_HEAD_DIM
DN_CONV = 5
DN_CHUNK = 64
ROPE_THETA = 10000.0
D_FF_DENSE = 2816
N_EXPERTS = 8
TOP_K = 2
D_FF_EXPERT = 3584
N_DENSE = (DEPTH + 1) // 2
N_MOE = DEPTH // 2
DEEPNORM_ALPHA = (2.0 * DEPTH) ** 0.25
DEEPNORM_BETA = (8.0 * DEPTH) ** -0.25
P_IN = 3 * NA_WIDTH + 4 * DN_WIDTH + 4 * DN_HEADS
LN_EPS = 1e-5
RMS_EPS = 1e-6

kernel_name = 'hybrid_natten_gdn_moe_diffusion_block'


def _layernorm(x, g, b):
    xf = x.astype(jnp.float32)
    mu = jnp.mean(xf, -1, keepdims=True)
    var = jnp.mean(jnp.square(xf - mu), -1, keepdims=True)
    return ((xf - mu) * lax.rsqrt(var + LN_EPS)).astype(x.dtype) * g + b


def _rmsnorm(x, g):
    xf = x.astype(jnp.float32)
    return (xf * lax.rsqrt(jnp.mean(xf * xf, -1, keepdims=True) + RMS_EPS)).astype(x.dtype) * g


def _l2norm(x):
    return x * lax.rsqrt(jnp.sum(x * x, -1, keepdims=True) + RMS_EPS)


def _modulate(x, shift, scale):
    return x * (1.0 + scale) + shift


def _axial_rope(x, rows, cols):
    half = x.shape[-1] // 2
    nf = half // 2
    inv = ROPE_THETA ** (-jnp.arange(nf, dtype=jnp.float32) / nf)

    def rot(xa, pos):
        ang = pos.astype(jnp.float32)[:, None] * inv
        cos = jnp.cos(ang)[None, :, None, :]
        sin = jnp.sin(ang)[None, :, None, :]
        x1, x2 = xa[..., :nf], xa[..., nf:]
        return jnp.concatenate([x1 * cos - x2 * sin, x1 * sin + x2 * cos], -1)

    return jnp.concatenate([rot(x[..., :half], rows), rot(x[..., half:], cols)], -1)


def _short_conv(x, w):
    return lax.conv_general_dilated(x, w[:, None, :].astype(x.dtype), window_strides=(1,), padding='SAME',
                                    dimension_numbers=('NWC', 'WIO', 'NWC'), feature_group_count=x.shape[-1])


def _neighbourhood_attention(q, k, v, kc, vc, rpb):
    B, N, H, d = q.shape
    rows = N // GRID_W
    kr = min(NA_KR, rows)
    n_cb = GRID_W // NA_KC
    kw = 2 * NA_KC
    nk = kr * kw
    scale = d ** -0.5
    qg = q.reshape(B, rows, n_cb, NA_KC, H, d)
    kg = k.reshape(B, rows, GRID_W, H, d)
    vg = v.reshape(B, rows, GRID_W, H, d)
    qcol = jnp.arange(GRID_W).reshape(n_cb, NA_KC)
    kstart = jnp.clip(jnp.arange(n_cb) * NA_KC - NA_KC // 2, 0, GRID_W - kw)
    kcol = kstart[:, None] + jnp.arange(kw)
    cstart = jnp.clip(qcol - NA_KC // 2, 0, GRID_W - NA_KC)
    col_ok = (kcol[:, None, :] >= cstart[..., None]) & (kcol[:, None, :] < cstart[..., None] + NA_KC)
    col_ok = jnp.broadcast_to(col_ok[:, :, None, :], (n_cb, NA_KC, kr, kw)).reshape(n_cb, NA_KC, nk)
    dc_idx = jnp.clip(kcol[:, None, :] - qcol[..., None] + NA_KC - 1, 0, 2 * NA_KC - 2)

    def row_block(r):
        rs = jnp.clip(r - kr // 2, 0, rows - kr)
        k_blk = lax.dynamic_slice_in_dim(kg, rs, kr, axis=1)[:, :, kcol]
        v_blk = lax.dynamic_slice_in_dim(vg, rs, kr, axis=1)[:, :, kcol]
        k_blk = jnp.swapaxes(k_blk, 1, 2).reshape(B, n_cb, nk, H, d)
        v_blk = jnp.swapaxes(v_blk, 1, 2).reshape(B, n_cb, nk, H, d)
        q_blk = lax.dynamic_index_in_dim(qg, r, axis=1, keepdims=False)
        dr_idx = rs + jnp.arange(kr) - r + NA_KR - 1
        bias = rpb[:, dr_idx[None, None, :, None], dc_idx[:, :, None, :]].reshape(H, n_cb, NA_KC, nk)
        s_loc = jnp.einsum('bjqhd,bjkhd->bhjqk', q_blk, k_blk).astype(jnp.float32) * scale + bias
        s_loc = jnp.where(col_ok, s_loc, -jnp.inf)
        s_ctx = jnp.einsum('bjqhd,bchd->bhjqc', q_blk, kc).astype(jnp.float32) * scale
        p = jax.nn.softmax(jnp.concatenate([s_loc, s_ctx], -1), axis=-1).astype(v.dtype)
        o = (jnp.einsum('bhjqk,bjkhd->bjqhd', p[..., :nk], v_blk)
             + jnp.einsum('bhjqc,bchd->bjqhd', p[..., nk:], vc))
        return o.reshape(B, GRID_W, H, d)

    o = lax.map(row_block, jnp.arange(rows))
    return jnp.moveaxis(o, 0, 1).reshape(B, N, H, d)


def _context_attention(q, k, v):
    s = jnp.einsum('bqhd,bkhd->bhqk', q, k).astype(jnp.float32) * (q.shape[-1] ** -0.5)
    p = jax.nn.softmax(s, axis=-1).astype(v.dtype)
    return jnp.einsum('bhqk,bkhd->bqhd', p, v)


def _gdn_chunked(q, k, v, g, beta, s0):
    B, T, H, _ = q.shape
    dv = v.shape[-1]
    nc = T // DN_CHUNK

    def blocks(a):
        return jnp.moveaxis(a.reshape((B, nc, DN_CHUNK, H) + a.shape[3:]), 3, 1)

    q, k, v, beta = blocks(q), blocks(k), blocks(v), blocks(beta)
    g = jnp.cumsum(blocks(g), axis=-1)
    k_beta = k * beta[..., None]
    v_beta = v * beta[..., None]
    idx = jnp.arange(DN_CHUNK)
    lower = idx[:, None] >= idx[None, :]
    strict = idx[:, None] > idx[None, :]
    decay = jnp.exp(jnp.where(lower, g[..., :, None] - g[..., None, :], -jnp.inf))
    a_mat = jnp.where(strict, jnp.einsum('bhncd,bhnsd->bhncs', k_beta, k) * decay, 0.0)
    eye = jnp.eye(DN_CHUNK, dtype=q.dtype)
    t_inv = lax.linalg.triangular_solve(a_mat + eye, jnp.broadcast_to(eye, a_mat.shape),
                                        left_side=True, lower=True, unit_diagonal=True)
    u = t_inv @ v_beta
    w = t_inv @ (k_beta * jnp.exp(g)[..., None])
    intra = jnp.einsum('bhncd,bhnsd->bhncs', q, k) * decay

    def step(S, inp):
        q_i, k_i, u_i, w_i, g_i, intra_i = inp
        v_new = u_i - w_i @ S
        o_i = (q_i * jnp.exp(g_i)[..., None]) @ S + intra_i @ v_new
        g_last = g_i[..., -1:]
        S = S * jnp.exp(g_last)[..., None] + jnp.einsum('bhcd,bhce->bhde', k_i * jnp.exp(g_last - g_i)[..., None], v_new)
        return S, o_i

    xs = (jnp.moveaxis(q, 2, 0), jnp.moveaxis(k, 2, 0), jnp.moveaxis(u, 2, 0),
          jnp.moveaxis(w, 2, 0), jnp.moveaxis(g, 2, 0), jnp.moveaxis(intra, 2, 0))
    s_final, o = lax.scan(step, s0, xs)
    o = jnp.moveaxis(jnp.moveaxis(o, 0, 2), 1, 3).reshape(B, T, H, dv)
    return o, s_final


def _dn_inputs(qkv, a, b, conv_w, a_log, dt_bias, pos):
    B, T, _ = qkv.shape
    qkv = jax.nn.silu(_short_conv(qkv, conv_w)).astype(jnp.float32).reshape(B, T, 3, DN_HEADS, DN_HEAD_DIM)
    q, k, v = _l2norm(qkv[:, :, 0]), _l2norm(qkv[:, :, 1]), qkv[:, :, 2]
    if pos is not None:
        q = _axial_rope(q, pos[0], pos[1])
        k = _axial_rope(k, pos[0], pos[1])
    q = q * DN_HEAD_DIM ** -0.5
    a = a.astype(jnp.float32).reshape(B, T, 2, DN_HEADS)
    b = b.astype(jnp.float32).reshape(B, T, 2, DN_HEADS)
    g = -jnp.exp(a_log.astype(jnp.float32)) * jax.nn.softplus(a + dt_bias.astype(jnp.float32))
    return q, k, v, g, jax.nn.sigmoid(b)


def _bidirectional_gdn(q, k, v, g, beta, s0_f, s0_b):
    flip = lambda t: jnp.flip(t, axis=1)
    o_f, s_f = _gdn_chunked(q, k, v, g[:, :, 0], beta[:, :, 0], s0_f)
    o_b, s_b = _gdn_chunked(flip(q), flip(k), flip(v), flip(g[:, :, 1]), flip(beta[:, :, 1]), s0_b)
    return o_f + flip(o_b), s_f, s_b


def _merge(o_na, o_dn, z, na_out_g, dn_norm_g, w_out):
    B, T = z.shape[0], z.shape[1]
    na = _rmsnorm(o_na.reshape(B, T, NA_WIDTH), na_out_g)
    dn = _rmsnorm(o_dn, dn_norm_g) * jax.nn.silu(z.astype(jnp.float32).reshape(B, T, DN_HEADS, DN_HEAD_DIM))
    return jnp.concatenate([na, dn.reshape(B, T, DN_WIDTH).astype(na.dtype)], -1) @ w_out


def _mixer(hl, hc, w_in, conv_w, a_log, dt_bias, rpb, na_out_g, dn_norm_g, w_out, ctx_out):
    B, N, _ = hl.shape
    splits = [NA_WIDTH, 2 * NA_WIDTH, 3 * NA_WIDTH, 3 * NA_WIDTH + 3 * DN_WIDTH,
              3 * NA_WIDTH + 4 * DN_WIDTH, 3 * NA_WIDTH + 4 * DN_WIDTH + 2 * DN_HEADS]
    qa_l, ka_l, va_l, qkv_l, z_l, b_l, a_l = jnp.split(hl @ w_in, splits, axis=-1)
    qa_c, ka_c, va_c, qkv_c, z_c, b_c, a_c = jnp.split(hc @ w_in, splits, axis=-1)
    heads = lambda t: t.reshape(t.shape[0], t.shape[1], NA_HEADS, NA_HEAD_DIM)
    kc, vc = heads(ka_c), heads(va_c)
    o_na_l = _neighbourhood_attention(heads(qa_l), heads(ka_l), heads(va_l), kc, vc, rpb)
    t = jnp.arange(N)
    q_c, k_c, v_c, g_c, beta_c = _dn_inputs(qkv_c, a_c, b_c, conv_w, a_log, dt_bias, None)
    q_l, k_l, v_l, g_l, beta_l = _dn_inputs(qkv_l, a_l, b_l, conv_w, a_log, dt_bias, (t // GRID_W, t % GRID_W))
    s0 = jnp.zeros((B, DN_HEADS, DN_HEAD_DIM, DN_HEAD_DIM), jnp.float32)
    o_dn_c, s_f, s_b = _bidirectional_gdn(q_c, k_c, v_c, g_c, beta_c, s0, s0)
    o_dn_l, _, _ = _bidirectional_gdn(q_l, k_l, v_l, g_l, beta_l, s_f, s_b)
    y_l = _merge(o_na_l, o_dn_l, z_l, na_out_g, dn_norm_g, w_out)
    if not ctx_out:
        return y_l, None
    o_na_c = _context_attention(heads(qa_c), kc, vc)
    y_c = _merge(o_na_c, o_dn_c, z_c, na_out_g, dn_norm_g, w_out)
    return y_l, y_c


def _swiglu(h, wg, wu, wd):
    return (jax.nn.silu(h @ wg) * (h @ wu)) @ wd


def _moe(h, router, wg, wu, wd):
    logits = (h @ router).astype(jnp.float32)
    top_v, top_i = lax.top_k(logits, TOP_K)
    top_w = jax.nn.softmax(top_v, axis=-1)
    gates = jnp.sum(jax.nn.one_hot(top_i, N_EXPERTS, dtype=jnp.float32) * top_w[..., None], axis=1).astype(h.dtype)
    y = jnp.zeros_like(h)
    for e in range(N_EXPERTS):
        y = y + gates[:, e:e + 1] * _swiglu(h, wg[e], wu[e], wd[e])
    return y


def setup_inputs(seed: int = 0) -> dict:
    key = jax.random.key(seed)
    ks = jax.random.split(key, 32)
    f32 = jnp.float32
    D = D_MODEL

    def nrm(k, shape, s):
        return jax.random.normal(k, shape, f32) * s

    dt = jnp.exp(jax.random.uniform(ks[9], (DEPTH, 2, DN_HEADS), f32, math.log(1e-3), math.log(1e-1)))
    return {
        'x': nrm(ks[0], (BATCH, SEQ, D), 1.0),
        'c': nrm(ks[1], (BATCH, D), 1.0),
        'ctx': nrm(ks[2], (BATCH, CTX_LEN, D), 1.0),
        'c_ctx': nrm(ks[3], (D,), 1.0),
        'w_mod': nrm(ks[4], (DEPTH, D, 6 * D), 0.5 * D ** -0.5),
        'b_mod': nrm(ks[5], (DEPTH, 6 * D), 0.02),
        'w_in': nrm(ks[6], (DEPTH, D, P_IN), D ** -0.5),
        'dn_conv_w': nrm(ks[7], (DEPTH, DN_CONV, 3 * DN_WIDTH), DN_CONV ** -0.5),
        'dn_a_log': jnp.log(jax.random.uniform(ks[8], (DEPTH, 2, DN_HEADS), f32, 1.0, 16.0)),
        'dn_dt_bias': dt + jnp.log(-jnp.expm1(-dt)),
        'dn_norm_g': 1.0 + nrm(ks[10], (DEPTH, DN_HEAD_DIM), 0.02),
        'na_rpb': nrm(ks[11], (DEPTH, NA_HEADS, 2 * NA_KR - 1, 2 * NA_KC - 1), 0.1),
        'na_out_g': 1.0 + nrm(ks[12], (DEPTH, NA_WIDTH), 0.02),
        'w_out': nrm(ks[13], (DEPTH, D_MIX, D), DEEPNORM_BETA * D_MIX ** -0.5),
        'ln1_g': 1.0 + nrm(ks[14], (DEPTH, D), 0.02),
        'ln1_b': nrm(ks[15], (DEPTH, D), 0.02),
        'ln2_g': 1.0 + nrm(ks[16], (DEPTH, D), 0.02),
        'ln2_b': nrm(ks[17], (DEPTH, D), 0.02),
        'ffn_w_gate': nrm(ks[18], (N_DENSE, D, D_FF_DENSE), D ** -0.5),
        'ffn_w_up': nrm(ks[19], (N_DENSE, D, D_FF_DENSE), D ** -0.5),
        'ffn_w_down': nrm(ks[20], (N_DENSE, D_FF_DENSE, D), DEEPNORM_BETA * D_FF_DENSE ** -0.5),
        'moe_router': nrm(ks[21], (N_MOE, D, N_EXPERTS), D ** -0.5),
        'moe_w_gate': nrm(ks[22], (N_MOE, N_EXPERTS, D, D_FF_EXPERT), D ** -0.5),
        'moe_w_up': nrm(ks[23], (N_MOE, N_EXPERTS, D, D_FF_EXPERT), D ** -0.5),
        'moe_w_down': nrm(ks[24], (N_MOE, N_EXPERTS, D_FF_EXPERT, D), DEEPNORM_BETA * D_FF_EXPERT ** -0.5),
    }


def reference(x, c, ctx, c_ctx, w_mod, b_mod, w_in, dn_conv_w, dn_a_log, dn_dt_bias, dn_norm_g, na_rpb,
              na_out_g, w_out, ln1_g, ln1_b, ln2_g, ln2_b, ffn_w_gate, ffn_w_up, ffn_w_down,
              moe_router, moe_w_gate, moe_w_up, moe_w_down):
    L = ctx.shape[1]
    xl, xc = x, ctx
    for l in range(DEPTH):
        ctx_out = l < DEPTH - 1
        m_l = (jax.nn.silu(c) @ w_mod[l] + b_mod[l])[:, None, :]
        m_c = (jax.nn.silu(c_ctx) @ w_mod[l] + b_mod[l])[None, None, :]
        sh1, sc1, g1, sh2, sc2, g2 = jnp.split(m_l, 6, axis=-1)
        csh1, csc1, cg1, csh2, csc2, cg2 = jnp.split(m_c, 6, axis=-1)
        y_l, y_c = _mixer(_modulate(xl, sh1, sc1), _modulate(xc, csh1, csc1), w_in[l], dn_conv_w[l],
                          dn_a_log[l], dn_dt_bias[l], na_rpb[l], na_out_g[l], dn_norm_g[l], w_out[l], ctx_out)
        xl = _layernorm(DEEPNORM_ALPHA * xl + g1 * y_l, ln1_g[l], ln1_b[l])
        if ctx_out:
            xc = _layernorm(DEEPNORM_ALPHA * xc + cg1 * y_c, ln1_g[l], ln1_b[l])
        h = _modulate(xl, sh2, sc2)
        if ctx_out:
            h = jnp.concatenate([_modulate(xc, csh2, csc2), h], axis=1)
        hf = h.reshape(-1, h.shape[-1])
        i = l // 2
        if l % 2 == 0:
            f = _swiglu(hf, ffn_w_gate[i], ffn_w_up[i], ffn_w_down[i])
        else:
            f = _moe(hf, moe_router[i], moe_w_gate[i], moe_w_up[i], moe_w_down[i])
        f = f.reshape(h.shape)
        if ctx_out:
            xc = _layernorm(DEEPNORM_ALPHA * xc + cg2 * f[:, :L], ln2_g[l], ln2_b[l])
            f = f[:, L:]
        xl = _layernorm(DEEPNORM_ALPHA * xl + g2 * f, ln2_g[l], ln2_b[l])
    return xl
```

```python
import numpy as np
from contextlib import ExitStack
import concourse.bass as bass
import concourse.mybir as mybir
from concourse.bass_utils import run_bass_kernel_spmd

F32 = mybir.dt.float32
BF16 = mybir.dt.bfloat16
AF = mybir.ActivationFunctionType
ALU = mybir.AluOpType
AX = mybir.AxisListType


class Buf:
    __slots__ = ("t", "name", "w", "r", "dsem", "dcnt")

    def __init__(self, t, name):
        self.t = t
        self.name = name
        self.w = None
        self.r = {}
        self.dsem = None
        self.dcnt = 0


class K:
    def __init__(self, debug_outs=()):
        self.nc = bass.Bass("TRN2", target_bir_lowering=False)
        self.es = ExitStack()
        nc = self.nc
        self.E = {"pe": nc.tensor, "act": nc.scalar, "dve": nc.vector, "pool": nc.gpsimd, "sp": nc.sync}
        self.sems = []
        self.esem = {}
        self.ecnt = {}
        for e in self.E:
            self.esem[e] = self.newsem("e_" + e)
            self.ecnt[e] = 0
        self.waited = {e: {} for e in self.E}
        self.dkeys = {}
        self.debug_outs = set(debug_outs)
        self.ninstr = 0
        self.dma_bufs = []
        self.stacks = [self.es]
        self.scope_bufs = [[]]
        self.free_sems = []
        self.semcount = {}

    def phase_begin(self):
        st = ExitStack()
        self.stacks.append(st)
        self.scope_bufs.append([])

    def phase_end(self):
        self.barrier_all()
        for b in self.scope_bufs.pop():
            if b.dsem is not None:
                self.semcount[b.dsem] = b.dcnt
                self.free_sems.append(b.dsem)
                self.dma_bufs.remove(b)
        self.stacks.pop().close()

    def barrier_all(self):
        need = {}
        for e in self.E:
            if self.ecnt[e]:
                need[self.esem[e]] = self.ecnt[e]
        for b in self.dma_bufs:
            need[b.dsem] = b.dcnt
        for e in self.E:
            self._wait(e, (dict(need), {}), is_dma=True)

    def newsem(self, name):
        s = self.es.enter_context(self.nc.semaphore(name))
        self.sems.append(s)
        return len(self.sems) - 1

    def sb(self, name, shape, dt):
        self.nalloc = getattr(self, "nalloc", 0) + 1
        t = self.stacks[-1].enter_context(self.nc.sbuf_tensor("s%d_" % self.nalloc + name, list(shape), dt))
        b = Buf(t, name)
        self.scope_bufs[-1].append(b)
        return b

    def ps(self, name, shape, dt):
        t = self.es.enter_context(self.nc.psum_tensor("p_" + name, list(shape), dt))
        return Buf(t, name)

    def dram_in(self, name, shape, dt):
        return self.nc.dram_tensor(name, list(shape), dt, kind="ExternalInput").ap()

    def dram_out(self, name, shape, dt):
        return self.nc.dram_tensor(name, list(shape), dt, kind="ExternalOutput").ap()

    def dram_scr(self, name, shape, dt):
        kind = "ExternalOutput" if name in self.debug_outs else "Internal"
        return self.nc.dram_tensor(name, list(shape), dt, kind=kind).ap()

    def dk(self, key):
        b = self.dkeys.get(key)
        if b is None:
            b = Buf(None, str(key))
            self.dkeys[key] = b
        return b

    def _need(self, reads, writes):
        need = {}
        hard = {}

        def add(d, tok):
            if tok is None:
                return
            s, v = tok
            if d.get(s, 0) < v:
                d[s] = v

        for b in reads:
            add(need, b.w)
            add(hard, b.w)
        for b in writes:
            add(need, b.w)
            for s, v in b.r.items():
                add(need, (s, v))
        return need, hard

    def _wait(self, e, nh, is_dma=False):
        need, hard = nh
        own = self.esem[e]
        wd = self.waited[e]
        for s, v in need.items():
            if s == own and not is_dma:
                if e == "pe" or hard.get(s, 0) < v:
                    v = hard.get(s, 0)
                    if e == "pe" or v == 0:
                        continue
            if wd.get(s, 0) >= v:
                continue
            self.E[e].wait_ge(self.sems[s], v)
            wd[s] = v
            self.ninstr += 1

    def _commit(self, tok, reads, writes):
        s, v = tok
        for b in reads:
            if b.r.get(s, 0) < v:
                b.r[s] = v
        for b in writes:
            b.w = tok
            b.r = {}

    def op(self, e, fn, reads=(), writes=()):
        self._wait(e, self._need(reads, writes))
        ins = fn(self.E[e])
        self.ecnt[e] += 1
        ins.then_inc(self.sems[self.esem[e]], 1)
        self.ninstr += 1
        self._commit((self.esem[e], self.ecnt[e]), reads, writes)

    def dma(self, q, out, in_, sbuf, reads=(), writes=(), **kw):
        if sbuf.dsem is None:
            if self.free_sems:
                sbuf.dsem = self.free_sems.pop()
                sbuf.dcnt = self.semcount[sbuf.dsem]
            else:
                sbuf.dsem = self.newsem("d%d_" % len(self.sems) + sbuf.name)
            self.dma_bufs.append(sbuf)
        self._wait(q, self._need(reads, writes), is_dma=True)
        sbuf.dcnt += 16
        self.E[q].dma_start(out=out, in_=in_, **kw).then_inc(self.sems[sbuf.dsem], 16)
        self.ninstr += 1
        self._commit((sbuf.dsem, sbuf.dcnt), reads, writes)

    def finish(self):
        need = {}
        for e in self.E:
            if self.ecnt[e]:
                need[self.esem[e]] = self.ecnt[e]
        for b in self.dma_bufs:
            need[b.dsem] = b.dcnt
        self._wait("sp", (need, {}), is_dma=True)
        while len(self.stacks) > 1:
            self.stacks.pop().close()
        self.es.close()


class Cfg:
    def __init__(s, NB=2, ROWS=64, CTX=256, DEPTH=4, FFD=2816, FFE=3584, NEXP=8, NCORES=8):
        s.NB, s.ROWS, s.CTX, s.DEPTH, s.FFD, s.FFE, s.NEXP, s.NCORES = NB, ROWS, CTX, DEPTH, FFD, FFE, NEXP, NCORES
        s.D = 1024
        s.GW = 64
        s.SEQ = ROWS * 64
        s.TOK = CTX + s.SEQ
        s.NT = s.TOK // 128
        s.NTC = CTX // 128
        s.ALPHA = (2.0 * DEPTH) ** 0.25
        s.PIN = 3600
        s.NV = 4
        assert NB + 1 <= 4

    def groups(s):
        g = []
        t = 0
        while t < s.CTX:
            n = min(512, s.CTX - t)
            g.append((t, n, True))
            t += n
        while t < s.TOK:
            n = min(512, s.TOK - t)
            g.append((t, n, False))
            t += n
        return g


def host_consts(cfg):
    import ml_dtypes
    c = {}
    c["identF"] = np.eye(128, dtype=np.float32)
    return c


def prep_core_inputs(cfg, inp, core):
    NB = cfg.NB
    b0 = core * NB
    L = cfg.DEPTH
    d = {}
    x0 = np.concatenate([inp["ctx"][b0:b0 + NB], inp["x"][b0:b0 + NB]], axis=1)
    d["x0"] = np.ascontiguousarray(x0, dtype=np.float32)
    cc = np.zeros((4, 1024), np.float32)
    cc[:NB] = inp["c"][b0:b0 + NB]
    cc[NB] = inp["c_ctx"]
    d["cT"] = np.ascontiguousarray(cc.reshape(4, 8, 128).transpose(2, 1, 0))
    d["w_mod"] = inp["w_mod"]
    d["bT"] = np.ascontiguousarray(inp["b_mod"].reshape(L, 48, 128).transpose(2, 0, 1))
    d["w_in"] = inp["w_in"]
    return d


def shared_inputs(cfg, inp):
    d = {}
    d.update(host_consts(cfg))
    d["BT"] = na_tables(cfg, inp["na_rpb"])
    d.update(gdn_consts(cfg))
    L = cfg.DEPTH
    d["cwT"] = np.ascontiguousarray(inp["dn_conv_w"].reshape(L, 5, 12, 128).transpose(3, 0, 2, 1))
    d["a_log"] = np.ascontiguousarray(inp["dn_a_log"].reshape(L, 8))
    for nm_ in ("na_out_g", "dn_norm_g", "w_out", "ln1_g", "ln1_b", "ln2_g", "ln2_b", "ffn_w_gate", "ffn_w_up", "ffn_w_down", "moe_router", "moe_w_gate", "moe_w_up", "moe_w_down"):
        d[nm_] = inp[nm_]
    d["dt_bias"] = np.ascontiguousarray(inp["dn_dt_bias"].reshape(L, 8))
    return d


class Prog:
    def __init__(self, cfg, debug_outs=(), stop_after=None):
        self.cfg = cfg
        self.k = K(debug_outs)
        self.stop_after = stop_after
        self.build()

    def build(self):
        cfg, k = self.cfg, self.k
        L, NB, TOK = cfg.DEPTH, cfg.NB, cfg.TOK
        I = self.I = {}
        I["x0"] = k.dram_in("x0", [NB, TOK, 1024], F32)
        I["cT"] = k.dram_in("cT", [128, 8, 4], F32)
        I["w_mod"] = k.dram_in("w_mod", [L, 1024, 6144], F32)
        I["bT"] = k.dram_in("bT", [128, L, 48], F32)
        I["w_in"] = k.dram_in("w_in", [L, 1024, 3600], F32)
        I["identF"] = k.dram_in("identF", [128, 128], F32)
        S = self.S = {}
        S["GROW"] = k.dram_scr("GROW", [L, 2, 4, 1024], F32)
        S["FT"] = k.dram_scr("FT", [NB, 2560, TOK], BF16)
        S["VA"] = k.dram_scr("VA", [NB, TOK, 8, 65], BF16)
        S["ZS"] = k.dram_scr("ZS", [NB, TOK, 512], BF16)
        S["BA"] = k.dram_scr("BA", [NB, TOK, 16], F32)
        S["ONA"] = k.dram_scr("ONA", [NB, TOK, 512], F32)
        S["X"] = k.dram_scr("X", [NB, TOK, 1024], F32)
        S["G16"] = k.dram_scr("G16", [NB, cfg.NT, 2, 128, 2048], BF16)
        S["G32"] = k.dram_scr("G32", [NB, cfg.NT, 2, 128, 520], F32)
        S["ODN"] = k.dram_scr("ODN", [NB, 2, TOK, 512], F32)
        I["gmask"] = k.dram_in("gmask", [128, 8, 128], F32)
        I["gmb"] = k.dram_in("gmb", [128, 4, 128], F32)
        I["ropec"] = k.dram_in("ropec", [128, cfg.NT - cfg.NTC, 64], F32)
        I["ropes"] = k.dram_in("ropes", [128, cfg.NT - cfg.NTC, 64], F32)
        I["cwT"] = k.dram_in("cwT", [128, L, 12, 5], F32)
        I["a_log"] = k.dram_in("a_log", [L, 8], F32)
        for nm_, sh in (("na_out_g", [L, 512]), ("dn_norm_g", [L, 128]), ("w_out", [L, 1024, 1024]), ("ln1_g", [L, 1024]), ("ln1_b", [L, 1024]),
                        ("ln2_g", [L, 1024]), ("ln2_b", [L, 1024]), ("ffn_w_gate", [(L + 1) // 2, 1024, cfg.FFD]), ("ffn_w_up", [(L + 1) // 2, 1024, cfg.FFD]),
                        ("ffn_w_down", [(L + 1) // 2, cfg.FFD, 1024]), ("moe_router", [L // 2, 1024, 8]), ("moe_w_gate", [L // 2, 8, 1024, cfg.FFE]),
                        ("moe_w_up", [L // 2, 8, 1024, cfg.FFE]), ("moe_w_down", [L // 2, 8, cfg.FFE, 1024])):
            I[nm_] = k.dram_in(nm_, sh, F32)
        I["dt_bias"] = k.dram_in("dt_bias", [L, 8], F32)
        self.NPL = len(na_plan(cfg)[1])
        I["BT"] = k.dram_in("BT", [L, 8, 64, self.NPL, 8, 64], F32)
        self.out = k.dram_out("y", [NB, cfg.SEQ, 1024], F32)
        self.P = [k.ps(f"pb{i}", [128, 512], F32) for i in range(8)]
        self.identF = k.sb("identF", [128, 128], F32)
        k.dma("sp", self.identF.t[:], I["identF"][:, :], self.identF, writes=[self.identF])
        self.identB = k.sb("identB", [128, 128], BF16)
        k.op("dve", lambda e: e.tensor_copy(out=self.identB.t[:], in_=self.identF.t[:]), reads=[self.identF], writes=[self.identB])
        self.modT = k.sb("modT", [128, L, 48, 4], F32)
        k.phase_begin()
        self.phase_mod()
        k.phase_end()
        if self.stop_after == "mod":
            return self.end_debug()
        for l in range(L):
            k.phase_begin()
            self.phase_proj(l)
            k.phase_end()
            if self.stop_after == ("proj", l):
                return self.end_debug()
            k.phase_begin()
            self.phase_na(l)
            k.phase_end()
            if self.stop_after == ("na", l):
                return self.end_debug()
            k.phase_begin()
            self.phase_gdn_a(l)
            k.phase_end()
            k.phase_begin()
            self.phase_gdn_b(l)
            k.phase_end()
            if self.stop_after == ("gdn", l):
                return self.end_debug()
            k.phase_begin()
            self.phase_merge(l)
            k.phase_end()
            if self.stop_after == ("merge", l):
                return self.end_debug()
            k.phase_begin()
            self.phase_ffn(l)
            k.phase_end()
            if self.stop_after == ("ffn", l):
                return self.end_debug()
        k.finish()

    def end_debug(self):
        self.k.finish()

    def phase_mod(self):
        cfg, k, I, S = self.cfg, self.k, self.I, self.S
        L = cfg.DEPTH
        cT = k.sb("cT", [128, 8, 4], F32)
        k.dma("sp", cT.t[:], I["cT"][:, :, :], cT, writes=[cT])
        sT = k.sb("sT", [128, 8, 4], F32)
        k.op("act", lambda e: e.activation(out=sT.t[:], in_=cT.t[:], func=AF.Silu), reads=[cT], writes=[sT])
        bT = k.sb("bT", [128, L, 48], F32)
        k.dma("sp", bT.t[:], I["bT"][:, :, :], bT, writes=[bT])
        modT = self.modT
        wbuf = [k.sb(f"wm{i}", [128, 8, 512], F32) for i in range(2)]
        grow = k.sb("grow", [4, 2048], F32)
        it = 0
        for l in range(L):
            for ng in range(12):
                wb = wbuf[it % 2]
                pm = self.P[it % 2]
                it += 1
                k.dma("sp", wb.t[:], I["w_mod"][l, :, ng * 512:(ng + 1) * 512].rearrange("(kc p) n -> p kc n", p=128), wb, writes=[wb])
                for c4 in range(4):
                    for kc in range(8):
                        k.op("pe", lambda e: e.matmul(pm.t[:, c4 * 4:(c4 + 1) * 4], wb.t[:, kc, c4 * 128:(c4 + 1) * 128], sT.t[:, kc, :],
                                                      start=(kc == 0), stop=(kc == 7)), reads=[wb, sT], writes=[pm])
                k.op("dve", lambda e: e.tensor_tensor(out=modT.t[:, l, ng * 4:(ng + 1) * 4, :],
                                                      in0=pm.t[:, 0:16].rearrange("p (c v) -> p c v", v=4),
                                                      in1=bT.t[:, l, ng * 4:(ng + 1) * 4].unsqueeze(2).broadcast_to([128, 4, 4]),
                                                      op=ALU.add), reads=[pm, bT], writes=[modT])
            for gi, c0 in enumerate((16, 40)):
                for half in range(2):
                    pb = self.P[2 + (gi * 2 + half) % 2]
                    for j in range(4):
                        c = c0 + half * 4 + j
                        k.op("pe", lambda e: e.transpose(out=pb.t[0:4, j * 128:(j + 1) * 128], in_=modT.t[:, l, c, :], identity=self.identF.t[:]),
                             reads=[modT, self.identF], writes=[pb])
                    k.op("dve", lambda e: e.tensor_copy(out=grow.t[:, (gi * 2 + half) * 512:(gi * 2 + half + 1) * 512], in_=pb.t[0:4, :]),
                         reads=[pb], writes=[grow])
            k.dma("sp", S["GROW"][l].rearrange("g v n -> v g n"), grow.t[:].rearrange("v (g n) -> v g n", g=2), grow,
                  reads=[grow], writes=[k.dk(("GROW", l))])
            for c0 in (8, 32):
                k.op("dve", lambda e: e.tensor_scalar(out=modT.t[:, l, c0:c0 + 8, :], in0=modT.t[:, l, c0:c0 + 8, :], scalar1=1.0, scalar2=None, op0=ALU.add),
                     reads=[], writes=[modT])

    def phase_proj(self, l):
        cfg, k, I, S = self.cfg, self.k, self.I, self.S
        NB = cfg.NB
        if True:
            self.win = k.sb("win", [128, 8, 3600], BF16)
            self.xt = [k.sb(f"xt{i}", [128, 1024], F32) for i in range(4)]
            self.hT = [k.sb(f"hT{i}", [128, 8, 512], BF16) for i in range(2)]
            self.ev = [k.sb(f"ev{i}", [128, 512], BF16) for i in range(4)]
            self.vaev = [k.sb(f"vaev{i}", [128, 8, 65], BF16) for i in range(2)]
            for b_ in self.vaev:
                k.op("pool", lambda e: e.memset(b_.t[:], 1.0), writes=[b_])
            self.zev = [k.sb(f"zev{i}", [128, 512], BF16) for i in range(2)]
            self.baev = [k.sb(f"baev{i}", [128, 16], F32) for i in range(2)]
            self.cnt = {"x": 0, "h": 0, "ev": 0, "tm": 0, "pb": 0}
        win = self.win
        for (c0, c1) in ((0, 1800), (1800, 3600)):
            k.dma("pool", win.t[:, :, c0:c1], I["w_in"][l, :, c0:c1].rearrange("(kc p) n -> p kc n", p=128), win, writes=[win])
        cnt = self.cnt
        for b in range(NB):
            for (t0, n, isctx) in cfg.groups():
                v = NB if isctx else b
                nt = n // 128
                hT = self.hT[cnt["h"] % 2]
                cnt["h"] += 1
                src = I["x0"] if l == 0 else S["X"]
                xts = []
                for i in range(nt):
                    xt = self.xt[cnt["x"] % 4]
                    cnt["x"] += 1
                    k.dma("sp", xt.t[:], src[b, t0 + i * 128:t0 + (i + 1) * 128, :], xt, reads=[k.dk(("X", b, (t0 // 128) + i))], writes=[xt])
                    xts.append(xt)
                for kc in range(8):
                    pb = self.P[cnt["pb"] % 8]
                    cnt["pb"] += 1
                    for i in range(nt):
                        k.op("pe", lambda e: e.transpose(out=pb.t[:, i * 128:(i + 1) * 128], in_=xts[i].t[:, kc * 128:(kc + 1) * 128], identity=self.identF.t[:]),
                             reads=[xts[i], self.identF], writes=[pb])
                    k.op("act", lambda e: e.activation(out=hT.t[:, kc, 0:n], in_=pb.t[:, 0:n], func=AF.Identity,
                                                       scale=self.modT.t[:, l, 8 + kc, v:v + 1], bias=self.modT.t[:, l, kc, v:v + 1]),
                         reads=[pb, self.modT], writes=[hT])
                for j in range(20):
                    col0 = j * 128 if j < 8 else 1536 + (j - 8) * 128
                    pb = self.P[cnt["pb"] % 8]
                    cnt["pb"] += 1
                    for kc in range(8):
                        k.op("pe", lambda e: e.matmul(pb.t[:, 0:n], win.t[:, kc, col0:col0 + 128], hT.t[:, kc, 0:n], start=(kc == 0), stop=(kc == 7)),
                             reads=[win, hT], writes=[pb])
                    ev = self.ev[cnt["ev"] % 4]
                    cnt["ev"] += 1
                    if j < 4:
                        k.op("act", lambda e: e.activation(out=ev.t[:, 0:n], in_=pb.t[:, 0:n], func=AF.Copy, scale=0.125), reads=[pb], writes=[ev])
                    elif j % 2 == 0:
                        k.op("act", lambda e: e.activation(out=ev.t[:, 0:n], in_=pb.t[:, 0:n], func=AF.Copy), reads=[pb], writes=[ev])
                    else:
                        k.op("dve", lambda e: e.tensor_copy(out=ev.t[:, 0:n], in_=pb.t[:, 0:n]), reads=[pb], writes=[ev])
                    k.dma("sp", S["FT"][b, j * 128:(j + 1) * 128, t0:t0 + n], ev.t[:, 0:n], ev, reads=[ev], writes=[k.dk(("FT", b, t0))])
                for i in range(nt):
                    ti = cnt["tm"] % 2
                    cnt["tm"] += 1
                    tok = slice(i * 128, (i + 1) * 128)
                    g0 = t0 + i * 128
                    pv, pz, pba = self.P[cnt["pb"] % 8], self.P[(cnt["pb"] + 1) % 8], self.P[(cnt["pb"] + 2) % 8]
                    cnt["pb"] += 3
                    for (pb, c0, w) in ((pv, 1024, 512), (pz, 3072, 512), (pba, 3584, 16)):
                        for kc in range(8):
                            k.op("pe", lambda e: e.matmul(pb.t[:, 0:w], hT.t[:, kc, tok], win.t[:, kc, c0:c0 + w], start=(kc == 0), stop=(kc == 7)),
                                 reads=[win, hT], writes=[pb])
                    va, ze, ba = self.vaev[ti], self.zev[ti], self.baev[ti]
                    k.op("dve", lambda e: e.tensor_copy(out=va.t[:, :, 0:64], in_=pv.t[:, :].rearrange("p (h d) -> p h d", d=64)), reads=[pv], writes=[va])
                    k.dma("sp", S["VA"][b, g0:g0 + 128], va.t[:], va, reads=[va], writes=[k.dk(("VA", b, g0))])
                    k.op("act", lambda e: e.activation(out=ze.t[:], in_=pz.t[:], func=AF.Silu), reads=[pz], writes=[ze])
                    k.dma("sp", S["ZS"][b, g0:g0 + 128, :], ze.t[:], ze, reads=[ze], writes=[k.dk(("ZS", b, g0))])
                    k.op("dve", lambda e: e.tensor_copy(out=ba.t[:], in_=pba.t[:, 0:16]), reads=[pba], writes=[ba])
                    k.dma("sp", S["BA"][b, g0:g0 + 128, :], ba.t[:], ba, reads=[ba], writes=[k.dk(("BA", b, g0))])


def na_plan(cfg):
    ROWS = cfg.ROWS
    rs = lambda r: min(max(r - 4, 0), ROWS - 8)
    planes = {}
    blocks = []
    for R in range(ROWS // 8):
        rows = list(range(8 * R, 8 * R + 8))
        lo, hi = rs(rows[0]), rs(rows[-1]) + 7
        krs = []
        for kr in range(lo, hi + 1):
            sig = tuple((kr - r + 7) if rs(r) <= kr <= rs(r) + 7 else None for r in rows)
            if sig not in planes:
                planes[sig] = len(planes)
            krs.append((kr, planes[sig], sig))
        blocks.append((lo, hi, krs))
    return blocks, planes


def na_tables(cfg, rpb):
    blocks, planes = na_plan(cfg)
    L = rpb.shape[0]
    NPL = len(planes)
    kc = np.arange(64)[:, None]
    qc = np.arange(64)[None, :]
    cstart = np.clip(qc - 8, 0, 48)
    col_ok = (kc >= cstart) & (kc < cstart + 16)
    dc = np.clip(kc - qc + 15, 0, 30)
    BT = np.full((L, 8, 64, NPL, 8, 64), -30000.0, np.float32)
    for sig, pi in planes.items():
        for ri, dr in enumerate(sig):
            if dr is None:
                continue
            vals = rpb[:, :, dr, :][:, :, dc]
            BT[:, :, :, pi, ri, :] = np.where(col_ok[None, None], vals, np.float32(-30000.0))
    return BT


def phase_na(self, l):
    cfg, k, I, S = self.cfg, self.k, self.I, self.S
    NB, CTX, NTC = cfg.NB, cfg.CTX, cfg.NTC
    blocks, planes = na_plan(cfg)
    NPL = len(planes)
    if True:
        self.bth = k.sb("bth", [64, NPL, 512], BF16)
        self.na_qt = [k.sb(f"naq{i}", [64, 512], BF16) for i in range(2)]
        self.na_kt = [k.sb(f"nak{i}", [64, 15 * 64], BF16) for i in range(2)]
        self.na_vt = [k.sb(f"nav{i}", [64, 15, 65], BF16) for i in range(2)]
        self.na_kc = [k.sb(f"nakc{i}", [64, CTX], BF16) for i in range(2)]
        self.na_vc = [k.sb(f"navc{i}", [128, NTC, 65], BF16) for i in range(2)]
        self.na_qc = [k.sb(f"naqc{i}", [64, CTX], BF16) for i in range(2)]
        self.na_pt = [[k.sb(f"napt{s}_{i}", [64, 512], BF16) for i in range(15)] for s in range(2)]
        self.na_pc = [[k.sb(f"napc{s}_{i}", [128, 512], BF16) for i in range(NTC)] for s in range(2)]
        self.na_rec = [k.sb(f"narec{i}", [128, 4], F32) for i in range(2)]
        self.na_o = [k.sb(f"nao{i}", [128, 4, 64], F32) for i in range(2)]
        self.nac = {"s": 0, "o": 0, "hb": 0, "blk": 0}
    c = self.nac
    SP = self.P[0:4]
    OP = self.P[4:6]

    def spb():
        c["s"] += 1
        return SP[c["s"] % 4]

    def attend(nq, qt_ap, locs, pcs, vc, dst_fn):
        ob = OP[c["o"] % 2]
        rec = self.na_rec[c["o"] % 2]
        ot = self.na_o[c["o"] % 2]
        c["o"] += 1
        ng = nq // 128
        for g in range(ng):
            ops = []
            for (pt, vt, ki, valid) in locs:
                if valid[g]:
                    ops.append((pt.t[:, g * 128:(g + 1) * 128], vt.t[:, ki, :], [pt, vt]))
            for ci, pc in enumerate(pcs):
                ops.append((pc.t[:, g * 128:(g + 1) * 128], vc.t[:, ci, :], [pc, vc]))
            for oi, (lh, rh, rd) in enumerate(ops):
                k.op("pe", lambda e: e.matmul(ob.t[:, g * 65:(g + 1) * 65], lh, rh, start=(oi == 0), stop=(oi == len(ops) - 1)), reads=rd, writes=[ob])
        ov = ob.t[:, 0:ng * 65].rearrange("p (g e) -> p g e", e=65)
        k.op("dve", lambda e: e.reciprocal(out=rec.t[:, 0:ng], in_=ov[:, :, 64]), reads=[ob], writes=[rec])
        k.op("dve", lambda e: e.tensor_tensor(out=ot.t[:, 0:ng, :], in0=ov[:, :, 0:64], in1=rec.t[:, 0:ng].unsqueeze(2).broadcast_to([128, ng, 64]), op=ALU.mult),
             reads=[ob, rec], writes=[ot])
        dst, dkey = dst_fn()
        k.dma("sp", dst, ot.t[:, 0:ng, :], ot, reads=[ot], writes=[dkey])

    for h in range(8):
        k.dma("pool", self.bth.t[:], I["BT"][l, h].rearrange("p n r q -> p n (r q)"), self.bth, writes=[self.bth])
        for b in range(NB):
            hb = c["hb"] % 2
            c["hb"] += 1
            kcT, vc, qcT = self.na_kc[hb], self.na_vc[hb], self.na_qc[hb]
            ftk = k.dk(("FT", b, 0))
            k.dma("sp", kcT.t[:], S["FT"][b, 512 + 64 * h:512 + 64 * h + 64, 0:CTX], kcT, reads=[ftk], writes=[kcT])
            k.dma("sp", qcT.t[:], S["FT"][b, 64 * h:64 * h + 64, 0:CTX], qcT, reads=[ftk], writes=[qcT])
            k.dma("sp", vc.t[:], S["VA"][b, 0:CTX, h, :].rearrange("(c p) e -> p c e", p=128), vc, reads=[k.dk(("VA", b, i * 128)) for i in range(NTC)], writes=[vc])
            st = c["blk"] % 2
            c["blk"] += 1
            pcs = self.na_pc[st]
            for ci in range(NTC):
                pb = spb()
                k.op("pe", lambda e: e.matmul(pb.t[:, 0:CTX], kcT.t[:, ci * 128:(ci + 1) * 128], qcT.t[:, :], start=True, stop=True), reads=[kcT, qcT], writes=[pb])
                k.op("act", lambda e: e.activation(out=pcs[ci].t[:, 0:CTX], in_=pb.t[:, 0:CTX], func=AF.Exp), reads=[pb], writes=[pcs[ci]])
            attend(CTX, None, [], pcs, vc,
                   lambda: (S["ONA"][b, 0:CTX, 64 * h:64 * h + 64].rearrange("(g p) d -> p g d", p=128), k.dk(("ONA", b, h, -1))))
            for R, (lo, hi, krs) in enumerate(blocks):
                st = c["blk"] % 2
                c["blk"] += 1
                qt, kt, vt = self.na_qt[st], self.na_kt[st], self.na_vt[st]
                nkr = hi - lo + 1
                q0 = CTX + R * 512
                k0 = CTX + lo * 64
                ft_keys = [k.dk(("FT", b, t0)) for (t0, n, isc) in cfg.groups()]
                va_keys = [k.dk(("VA", b, (k0 // 128) * 128 + i * 128)) for i in range((nkr * 64 + 127) // 128 + 1)]
                k.dma("sp", qt.t[:], S["FT"][b, 64 * h:64 * h + 64, q0:q0 + 512], qt, reads=ft_keys, writes=[qt])
                k.dma("sp", kt.t[:, 0:nkr * 64], S["FT"][b, 512 + 64 * h:512 + 64 * h + 64, k0:k0 + nkr * 64], kt, reads=ft_keys, writes=[kt])
                k.dma("sp", vt.t[:, 0:nkr, :], S["VA"][b, k0:k0 + nkr * 64, h, :].rearrange("(r p) e -> p r e", p=64), vt, reads=va_keys, writes=[vt])
                pts = self.na_pt[st]
                pcs = self.na_pc[st]
                locs = []
                for ki, (kr, pi, sig) in enumerate(krs):
                    pb = spb()
                    k.op("pe", lambda e: e.matmul(pb.t[0:64, :], kt.t[:, ki * 64:(ki + 1) * 64], qt.t[:, :], start=True, stop=False), reads=[kt, qt], writes=[pb])
                    k.op("pe", lambda e: e.matmul(pb.t[0:64, :], self.identB.t[0:64, 0:64], self.bth.t[:, pi, :], start=False, stop=True), reads=[self.identB, self.bth], writes=[pb])
                    k.op("act", lambda e: e.activation(out=pts[ki].t[:, :], in_=pb.t[0:64, :], func=AF.Exp), reads=[pb], writes=[pts[ki]])
                    valid = [(sig[2 * g] is not None) or (sig[2 * g + 1] is not None) for g in range(4)]
                    locs.append((pts[ki], vt, ki, valid))
                for ci in range(NTC):
                    pb = spb()
                    k.op("pe", lambda e: e.matmul(pb.t[:, :], kcT.t[:, ci * 128:(ci + 1) * 128], qt.t[:, :], start=True, stop=True), reads=[kcT, qt], writes=[pb])
                    k.op("act", lambda e: e.activation(out=pcs[ci].t[:, :], in_=pb.t[:, :], func=AF.Exp), reads=[pb], writes=[pcs[ci]])
                attend(512, None, locs, pcs, vc,
                       lambda: (S["ONA"][b, q0:q0 + 512, 64 * h:64 * h + 64].rearrange("(g p) d -> p g d", p=128), k.dk(("ONA", b, h, R))))


Prog.phase_na = phase_na


def gdn_consts(cfg):
    idx = np.arange(128)
    same = (idx[:, None] // 64) == (idx[None, :] // 64)
    c = {}
    Ef = same & (idx[:, None] <= idx[None, :])
    Eb = same & (idx[:, None] >= idx[None, :])
    Ff = same & (idx[:, None] > idx[None, :])
    Fb = same & (idx[:, None] < idx[None, :])
    sel0 = np.broadcast_to((idx[:, None] // 64) == 0, (128, 128))
    sel1 = np.broadcast_to((idx[:, None] // 64) == 1, (128, 128))
    gm = np.stack([Ef, Eb, Ff, Fb, sel0, sel1, np.ones((128, 128), bool), np.zeros((128, 128), bool)], 1).astype(np.float32)
    c["gmask"] = np.ascontiguousarray(gm)
    mb = np.stack([(Ef.astype(np.float32) - 1.0) * 30000.0, (Eb.astype(np.float32) - 1.0) * 30000.0,
                   -(Ef & ~np.eye(128, dtype=bool)).astype(np.float32), -(Eb & ~np.eye(128, dtype=bool)).astype(np.float32)], 1)
    c["gmb"] = np.ascontiguousarray(mb.astype(np.float32))
    nf = 32
    inv = (10000.0 ** (-np.arange(nf, dtype=np.float32) / nf)).astype(np.float32)
    t = np.arange(cfg.SEQ)
    rows = (t // 64).astype(np.float32)
    cols = (t % 64).astype(np.float32)
    ang = np.concatenate([rows[:, None] * inv[None, :], cols[:, None] * inv[None, :]], 1).astype(np.float32)
    NTL = cfg.SEQ // 128
    c["ropec"] = np.ascontiguousarray(np.cos(ang).astype(np.float32).reshape(NTL, 128, 64).transpose(1, 0, 2))
    c["ropes"] = np.ascontiguousarray(np.sin(ang).astype(np.float32).reshape(NTL, 128, 64).transpose(1, 0, 2))
    return c


def phase_gdn_a(self, l):
    cfg, k, I, S = self.cfg, self.k, self.I, self.S
    NB, CTX, NTC, NT = cfg.NB, cfg.CTX, cfg.NTC, cfg.NT
    NTL = NT - NTC
    P = self.P
    pc = {"i": 0}

    def nextP():
        pc["i"] += 1
        return P[pc["i"] % 7]

    gm = k.sb("gm", [128, 8, 128], F32)
    k.dma("sp", gm.t[:], I["gmask"][:, :, :], gm, writes=[gm])
    gmb = k.sb("gmb", [128, 4, 128], F32)
    k.dma("sp", gmb.t[:], I["gmb"][:, :, :], gmb, writes=[gmb])
    ropec = k.sb("ropec", [128, NTL, 64], F32)
    ropes = k.sb("ropes", [128, NTL, 64], F32)
    k.dma("sp", ropec.t[:], I["ropec"][:, :, :], ropec, writes=[ropec])
    k.dma("sp", ropes.t[:], I["ropes"][:, :, :], ropes, writes=[ropes])
    cw = k.sb("cw", [128, 12, 5], F32)
    k.dma("sp", cw.t[:], I["cwT"][:, l], cw, writes=[cw])
    dw = k.sb("dw", [128, 12, 5, 128], BF16)
    for cc in range(12):
        for s in range(5):
            k.op("dve", lambda e: e.tensor_scalar(out=dw.t[:, cc, s, :], in0=self.identF.t[:], scalar1=cw.t[:, cc, s:s + 1], scalar2=None, op0=ALU.mult),
                 reads=[cw, self.identF], writes=[dw])
    alog = k.sb("alog", [128, 8], F32)
    dtb = k.sb("dtb", [128, 8], F32)
    k.dma("sp", alog.t[:], I["a_log"][l:l + 1, :].partition_broadcast(128) if False else I["a_log"][l:l + 1, :].broadcast_to([128, 8]), alog, writes=[alog])
    k.dma("sp", dtb.t[:], I["dt_bias"][l:l + 1, :].broadcast_to([128, 8]), dtb, writes=[dtb])
    nea = k.sb("nea", [128, 8], F32)
    k.op("act", lambda e: e.activation(out=nea.t[:], in_=alog.t[:], func=AF.Exp), reads=[alog], writes=[nea])
    k.op("dve", lambda e: e.tensor_scalar(out=nea.t[:], in0=nea.t[:], scalar1=-1.0, scalar2=None, op0=ALU.mult), reads=[nea], writes=[nea])

    xc = [k.sb(f"xc{i}", [128, 12, 516], BF16) for i in range(2)]
    sil = [k.sb(f"sil{i}", [128, 512], BF16) for i in range(2)]
    tma = [k.sb(f"tma{i}", [128, 4, 12, 128], F32) for i in range(1)]
    NS = 1

    def mk(name, shape, dt, n=NS):
        return [k.sb(f"{name}{i}", shape, dt) for i in range(n)]

    sqt = mk("sqt", [128, 8, 128], F32, 1)
    ss = mk("ss", [128, 8], F32)
    qkn = mk("qkn", [128, 8, 128], F32)
    qkr = mk("qkr", [128, 8, 128], F32)
    rt1 = mk("rt1", [128, 8, 2, 32], F32, 1)
    rt2 = mk("rt2", [128, 8, 2, 32], F32, 1)
    qkb = mk("qkb", [128, 8, 128], BF16)
    bat = mk("bat", [128, 16], F32)
    gsm = mk("gsm", [128, 6, 8], F32)
    ex = mk("ex", [128, 32], F32)
    tok16 = mk("tok16", [128, 5, 8, 128], BF16)
    ft16 = mk("ft16", [128, 24, 128], BF16)
    rhs1 = mk("rhs1", [128, 8, 128], F32)
    rhs2 = mk("rhs2", [128, 8, 128], F32)
    dtmp = mk("dtmp", [128, 4, 128], F32)
    dec = mk("dec", [128, 4, 128], F32)
    decn = mk("decn", [128, 4, 128], F32)
    mt = mk("mt", [128, 4, 128], BF16, 3)
    mm = mk("mm", [128, 4, 128], BF16, 3)
    rt = mk("rt", [128, 4, 128], BF16, 3)
    g16 = mk("g16", [128, 4, 4, 128], BF16, 2)
    g32 = mk("g32", [128, 520], F32, 2)
    cn = {"g": 0, "t": 0, "u": 0, "m": 0}

    for b in range(NB):
        for (t0, n, isctx) in cfg.groups():
            s0, s1 = (0, CTX) if isctx else (CTX, cfg.TOK)
            nt = n // 128
            gi = cn["g"] % 2
            cn["g"] += 1
            xcb = xc[gi]
            lo, hi = max(t0 - 2, s0), min(t0 + n + 2, s1)
            if lo != t0 - 2 or hi != t0 + n + 2:
                k.op("pool", lambda e: e.memset(xcb.t[:], 0.0), writes=[xcb])
            k.dma("sp", xcb.t[:, :, lo - (t0 - 2):hi - (t0 - 2)], S["FT"][b, 1024:2560, lo:hi].rearrange("(c p) t -> p c t", p=128), xcb,
                  reads=[k.dk(("FT", b, g_[0])) for g_ in cfg.groups()], writes=[xcb])
            tm = tma[0]
            for cc in range(12):
                pb = nextP()
                for s in range(5):
                    k.op("pe", lambda e: e.matmul(pb.t[:, 0:n], dw.t[:, cc, s, :], xcb.t[:, cc, s:s + n], start=(s == 0), stop=(s == 4)), reads=[dw, xcb], writes=[pb])
                sl = sil[cc % 2]
                k.op("act", lambda e: e.activation(out=sl.t[:, 0:n], in_=pb.t[:, 0:n], func=AF.Silu), reads=[pb], writes=[sl])
                pt = nextP()
                ptb = pt.t[:].bitcast(BF16)
                for i in range(nt):
                    k.op("pe", lambda e: e.transpose(out=ptb[:, i * 128:(i + 1) * 128], in_=sl.t[:, i * 128:(i + 1) * 128], identity=self.identB.t[:]),
                         reads=[sl, self.identB], writes=[pt])
                k.op("dve", lambda e: e.tensor_copy(out=tm.t[:, 0:nt, cc, :], in_=ptb[:, 0:nt * 128].rearrange("p (i d) -> p i d", d=128)), reads=[pt], writes=[tm])
            for i in range(nt):
                ti = t0 // 128 + i
                u = cn["t"] % NS
                cn["t"] += 1
                T = tm.t[:, i]
                k.op("dve", lambda e: e.tensor_tensor(out=sqt[0].t[:], in0=T[:, 0:8, :], in1=T[:, 0:8, :], op=ALU.mult), reads=[tm], writes=[sqt[0]])
                k.op("dve", lambda e: e.tensor_reduce(out=ss[u].t[:], in_=sqt[0].t[:], axis=AX.X, op=ALU.add), reads=[sqt[0]], writes=[ss[u]])
                k.op("dve", lambda e: e.tensor_scalar(out=ss[u].t[:], in0=ss[u].t[:], scalar1=1e-6, scalar2=None, op0=ALU.add), reads=[ss[u]], writes=[ss[u]])
                k.op("act", lambda e: e.activation(out=ss[u].t[:], in_=ss[u].t[:], func=AF.Sqrt), reads=[ss[u]], writes=[ss[u]])
                k.op("dve", lambda e: e.reciprocal(out=ss[u].t[:], in_=ss[u].t[:]), reads=[ss[u]], writes=[ss[u]])
                k.op("dve", lambda e: e.tensor_scalar(out=ss[u].t[:, 0:4], in0=ss[u].t[:, 0:4], scalar1=float(128 ** -0.5), scalar2=None, op0=ALU.mult), reads=[ss[u]], writes=[ss[u]])
                k.op("dve", lambda e: e.tensor_tensor(out=qkn[u].t[:], in0=T[:, 0:8, :], in1=ss[u].t[:].unsqueeze(2).broadcast_to([128, 8, 128]), op=ALU.mult),
                     reads=[tm, ss[u]], writes=[qkn[u]])
                if isctx:
                    qk_ = qkn[u]
                else:
                    tl = ti - NTC
                    x5 = qkn[u].t[:].rearrange("p h (a c f) -> p h a c f", a=2, c=2)
                    o5 = qkr[u].t[:].rearrange("p h (a c f) -> p h a c f", a=2, c=2)
                    cosb = ropec.t[:, tl, :].rearrange("p (a f) -> p a f", a=2).unsqueeze(1).broadcast_to([128, 8, 2, 32])
                    sinb = ropes.t[:, tl, :].rearrange("p (a f) -> p a f", a=2).unsqueeze(1).broadcast_to([128, 8, 2, 32])
                    xA, xB = x5[:, :, :, 0, :], x5[:, :, :, 1, :]
                    k.op("dve", lambda e: e.tensor_tensor(out=rt1[0].t[:], in0=xA, in1=cosb, op=ALU.mult), reads=[qkn[u], ropec], writes=[rt1[0]])
                    k.op("dve", lambda e: e.tensor_tensor(out=rt2[0].t[:], in0=xB, in1=sinb, op=ALU.mult), reads=[qkn[u], ropes], writes=[rt2[0]])
                    k.op("dve", lambda e: e.tensor_tensor(out=o5[:, :, :, 0, :], in0=rt1[0].t[:], in1=rt2[0].t[:], op=ALU.subtract), reads=[rt1[0], rt2[0]], writes=[qkr[u]])
                    k.op("dve", lambda e: e.tensor_tensor(out=rt1[0].t[:], in0=xA, in1=sinb, op=ALU.mult), reads=[qkn[u], ropes], writes=[rt1[0]])
                    k.op("dve", lambda e: e.tensor_tensor(out=rt2[0].t[:], in0=xB, in1=cosb, op=ALU.mult), reads=[qkn[u], ropec], writes=[rt2[0]])
                    k.op("dve", lambda e: e.tensor_tensor(out=o5[:, :, :, 1, :], in0=rt1[0].t[:], in1=rt2[0].t[:], op=ALU.add), reads=[rt1[0], rt2[0]], writes=[qkr[u]])
                    qk_ = qkr[u]
                k.op("act", lambda e: e.activation(out=qkb[u].t[:], in_=qk_.t[:], func=AF.Copy), reads=[qk_], writes=[qkb[u]])
                k.dma("sp", bat[u].t[:], S["BA"][b, ti * 128:(ti + 1) * 128, :], bat[u], reads=[k.dk(("BA", b, ti * 128))], writes=[bat[u]])
                G = gsm[u]
                beta, xx, nx, lp, g, bsc = [G.t[:, j, :] for j in range(6)]
                k.op("act", lambda e: e.activation(out=beta, in_=bat[u].t[:, 0:8], func=AF.Sigmoid), reads=[bat[u]], writes=[G])
                k.op("dve", lambda e: e.tensor_tensor(out=xx, in0=bat[u].t[:, 8:16], in1=dtb.t[:], op=ALU.add), reads=[bat[u], dtb], writes=[G])
                k.op("dve", lambda e: e.tensor_scalar(out=lp, in0=xx, scalar1=0.0, scalar2=None, op0=ALU.max), reads=[G], writes=[G])
                k.op("dve", lambda e: e.scalar_tensor_tensor(out=nx, in0=lp, scalar=-2.0, in1=xx, op0=ALU.mult, op1=ALU.add), reads=[G], writes=[G])
                k.op("act", lambda e: e.activation(out=nx, in_=nx, func=AF.Exp), reads=[G], writes=[G])
                k.op("act", lambda e: e.activation(out=nx, in_=nx, func=AF.Ln, bias=1.0), reads=[G], writes=[G])
                k.op("dve", lambda e: e.tensor_tensor(out=lp, in0=lp, in1=nx, op=ALU.add), reads=[G], writes=[G])
                k.op("dve", lambda e: e.tensor_tensor(out=g, in0=lp, in1=nea.t[:], op=ALU.mult), reads=[G, nea], writes=[G])
                pg = nextP()
                for d in range(2):
                    k.op("pe", lambda e: e.matmul(pg.t[:, d * 4:(d + 1) * 4], gm.t[:, d, :], G.t[:, 4, d * 4:(d + 1) * 4], start=True, stop=True), reads=[gm, G], writes=[pg])
                    k.op("pe", lambda e: e.matmul(pg.t[:, 8 + d * 4:8 + (d + 1) * 4], gm.t[:, 2 + d, :], G.t[:, 4, d * 4:(d + 1) * 4], start=True, stop=True), reads=[gm, G], writes=[pg])
                for c in range(2):
                    k.op("pe", lambda e: e.matmul(pg.t[:, 16 + c * 8:16 + (c + 1) * 8], gm.t[:, 4 + c, :], G.t[:, 4, :], start=True, stop=True), reads=[gm, G], writes=[pg])
                EX = ex[u]
                k.op("act", lambda e: e.activation(out=EX.t[:], in_=pg.t[:, 0:32], func=AF.Exp), reads=[pg], writes=[EX])
                k.op("dve", lambda e: e.tensor_tensor(out=bsc, in0=beta, in1=EX.t[:, 0:8], op=ALU.mult), reads=[G, EX], writes=[G])
                T16 = tok16[u]
                Kh = qk_.t[:, 4:8, :].unsqueeze(1).broadcast_to([128, 2, 4, 128])
                Qh = qk_.t[:, 0:4, :].unsqueeze(1).broadcast_to([128, 2, 4, 128])
                Vh = T[:, 8:12, :].unsqueeze(1).broadcast_to([128, 2, 4, 128])

                def sc8(ap):
                    return ap.rearrange("p (d h) -> p d h", d=2).unsqueeze(3).broadcast_to([128, 2, 4, 128])

                for j, (src, scl, rd) in enumerate(((Kh, beta, [G]), (Kh, bsc, [G]), (Kh, EX.t[:, 8:16], [EX]), (Vh, beta, [G, tm]), (Qh, EX.t[:, 0:8], [EX]))):
                    eng = "dve" if j % 2 == 0 else "pool"
                    k.op(eng, lambda e: e.tensor_tensor(out=T16.t[:, j].rearrange("p (d h) x -> p d h x", d=2), in0=src, in1=sc8(scl), op=ALU.mult),
                         reads=[qk_] + rd, writes=[T16])
                F16 = ft16[u]
                srcs = [qkb[u].t[:, 4 + h, :] for h in range(4)] + [qkb[u].t[:, h, :] for h in range(4)] + [T16.t[:, 0, j, :] for j in range(8)] + [T16.t[:, 4, j, :] for j in range(8)]
                for q8 in range(3):
                    pt = nextP()
                    ptb = pt.t[:].bitcast(BF16)
                    for j in range(8):
                        k.op("pe", lambda e: e.transpose(out=ptb[:, j * 128:(j + 1) * 128], in_=srcs[q8 * 8 + j], identity=self.identB.t[:]),
                             reads=[qkb[u], T16, self.identB], writes=[pt])
                    eng = "act" if q8 % 2 == 0 else "dve"
                    if eng == "act":
                        k.op("act", lambda e: e.activation(out=F16.t[:, q8 * 8:(q8 + 1) * 8, :], in_=ptb.rearrange("p (j x) -> p j x", x=128), func=AF.Copy), reads=[pt], writes=[F16])
                    else:
                        k.op("dve", lambda e: e.tensor_copy(out=F16.t[:, q8 * 8:(q8 + 1) * 8, :], in_=ptb.rearrange("p (j x) -> p j x", x=128)), reads=[pt], writes=[F16])
                R1, R2 = rhs1[u], rhs2[u]
                for d in range(2):
                    k.op("pool", lambda e: e.tensor_tensor(out=R1.t[:, d * 4:(d + 1) * 4, :], in0=gm.t[:, d, :].unsqueeze(1).broadcast_to([128, 4, 128]),
                                                           in1=G.t[:, 4, d * 4:(d + 1) * 4].unsqueeze(2).broadcast_to([128, 4, 128]), op=ALU.mult), reads=[gm, G], writes=[R1])
                k.op("act", lambda e: e.activation(out=R2.t[:], in_=G.t[:, 4, :].unsqueeze(2).broadcast_to([128, 8, 128]), func=AF.Copy, scale=-1.0), reads=[G], writes=[R2])
                pqk = P[7]
                for h in range(4):
                    k.op("pe", lambda e: e.matmul(pqk.t[:, h * 128:(h + 1) * 128], F16.t[:, h, :], F16.t[:, 4 + h, :], start=True, stop=True), reads=[F16], writes=[pqk])
                for d in range(2):
                    mu = 0
                    gu = cn["u"] % 2
                    cn["u"] += 1
                    pgb = nextP()
                    for h in range(4):
                        k.op("pe", lambda e: e.matmul(pgb.t[:, h * 128:(h + 1) * 128], F16.t[:, h, :], F16.t[:, 8 + d * 4 + h, :], start=True, stop=True), reads=[F16], writes=[pgb])
                    pd = nextP()
                    k.op("pe", lambda e: e.matmul(pd.t[:, :], gm.t[:, 6, :], R1.t[:, d * 4:(d + 1) * 4, :], start=True, stop=False), reads=[gm, R1], writes=[pd])
                    k.op("pe", lambda e: e.matmul(pd.t[:, :], gm.t[:, d, :], R2.t[:, d * 4:(d + 1) * 4, :], start=False, stop=True), reads=[gm, R2], writes=[pd])
                    DT, DE, DN = dtmp[mu], dec[mu], decn[mu]
                    k.op("dve", lambda e: e.scalar_tensor_tensor(out=DT.t[:], in0=pd.t[:, :].rearrange("p (h x) -> p h x", x=128), scalar=0.0,
                                                                 in1=gmb.t[:, d, :].unsqueeze(1).broadcast_to([128, 4, 128]), op0=ALU.min, op1=ALU.add), reads=[pd, gmb], writes=[DT])
                    k.op("act", lambda e: e.activation(out=DE.t[:], in_=DT.t[:], func=AF.Exp), reads=[DT], writes=[DE])
                    k.op("pool", lambda e: e.tensor_tensor(out=DN.t[:], in0=DE.t[:], in1=gmb.t[:, 2 + d, :].unsqueeze(1).broadcast_to([128, 4, 128]), op=ALU.mult), reads=[DE, gmb], writes=[DN])
                    G16, G32 = g16[gu], g32[gu]
                    def nm():
                        cn["m"] += 1
                        return cn["m"] % 3
                    MT = mt[nm()]
                    k.op("dve", lambda e: e.tensor_tensor(out=MT.t[:], in0=pgb.t[:, :].rearrange("p (h x) -> p h x", x=128), in1=DN.t[:], op=ALU.mult), reads=[pgb, DN], writes=[MT])
                    k.op("dve", lambda e: e.tensor_tensor(out=G16.t[:, 1], in0=pqk.t[:, :].rearrange("p (h x) -> p h x", x=128), in1=DE.t[:], op=ALU.mult), reads=[pqk, DE], writes=[G16])
                    pt = nextP()
                    ptb = pt.t[:].bitcast(BF16)
                    for h in range(4):
                        k.op("pe", lambda e: e.transpose(out=ptb[:, h * 128:(h + 1) * 128], in_=MT.t[:, h, :], identity=self.identB.t[:]), reads=[MT, self.identB], writes=[pt])
                    M = mm[cn["m"] % 3]
                    k.op("act", lambda e: e.activation(out=M.t[:], in_=ptb[:, 0:512].rearrange("p (h x) -> p h x", x=128), func=AF.Copy), reads=[pt], writes=[M])
                    RT = rt[cn["m"] % 3]
                    k.op("pool", lambda e: e.tensor_tensor(out=RT.t[:], in0=MT.t[:], in1=self.identB.t[:].unsqueeze(1).broadcast_to([128, 4, 128]), op=ALU.add), reads=[MT, self.identB], writes=[RT])
                    for step in range(5):
                        p1 = nextP()
                        for h in range(4):
                            k.op("pe", lambda e: e.matmul(p1.t[:, h * 128:(h + 1) * 128], MT.t[:, h, :], M.t[:, h, :], start=True, stop=True), reads=[MT, M], writes=[p1])
                        if step < 4:
                            p2 = nextP()
                            for h in range(4):
                                k.op("pe", lambda e: e.matmul(p2.t[:, h * 128:(h + 1) * 128], M.t[:, h, :], MT.t[:, h, :], start=True, stop=True), reads=[MT, M], writes=[p2])
                        ni = nm()
                        M2, MT2, RT2 = mm[ni], mt[ni], rt[ni]
                        k.op("act", lambda e: e.activation(out=M2.t[:], in_=p1.t[:, :].rearrange("p (h x) -> p h x", x=128), func=AF.Copy), reads=[p1], writes=[M2])
                        if step < 4:
                            k.op("dve", lambda e: e.tensor_copy(out=MT2.t[:], in_=p2.t[:, :].rearrange("p (h x) -> p h x", x=128)), reads=[p2], writes=[MT2])
                        p3 = nextP()
                        for h in range(4):
                            k.op("pe", lambda e: e.matmul(p3.t[:, h * 128:(h + 1) * 128], M2.t[:, h, :], RT.t[:, h, :], start=True, stop=True), reads=[M2, RT], writes=[p3])
                        k.op("dve", lambda e: e.tensor_tensor(out=RT2.t[:], in0=p3.t[:, :].rearrange("p (h x) -> p h x", x=128), in1=RT.t[:], op=ALU.add), reads=[p3, RT], writes=[RT2])
                        M, MT, RT = M2, MT2, RT2
                    pw = nextP()
                    for h in range(4):
                        k.op("pe", lambda e: e.matmul(pw.t[:, h * 128:(h + 1) * 128], T16.t[:, 1, d * 4 + h, :], RT.t[:, h, :], start=True, stop=True), reads=[T16, RT], writes=[pw])
                    k.op("act", lambda e: e.activation(out=G16.t[:, 0], in_=pw.t[:, :].rearrange("p (h x) -> p h x", x=128), func=AF.Copy, scale=-1.0), reads=[pw], writes=[G16])
                    pu = nextP()
                    for h in range(4):
                        k.op("pe", lambda e: e.matmul(pu.t[:, h * 128:(h + 1) * 128], RT.t[:, h, :], T16.t[:, 3, d * 4 + h, :], start=True, stop=True), reads=[T16, RT], writes=[pu])
                    k.op("dve", lambda e: e.tensor_copy(out=G32.t[:, 0:512], in_=pu.t[:, :]), reads=[pu], writes=[G32])
                    k.op("pool", lambda e: e.tensor_copy(out=G16.t[:, 2], in_=F16.t[:, 16 + d * 4:16 + d * 4 + 4, :]), reads=[F16], writes=[G16])
                    k.op("pool", lambda e: e.tensor_copy(out=G16.t[:, 3], in_=T16.t[:, 2, d * 4:d * 4 + 4, :]), reads=[T16], writes=[G16])
                    k.op("dve", lambda e: e.tensor_copy(out=G32.t[:, 512:520].rearrange("p (c h) -> p c h", c=2), in_=EX.t[:, 16:32].rearrange("p (c j) -> p c j", c=2)[:, :, d * 4:(d + 1) * 4]),
                         reads=[EX], writes=[G32])
                    k.dma("sp", S["G16"][b, ti, d], G16.t[:].rearrange("p a h x -> p (a h x)"), G16, reads=[G16], writes=[k.dk(("G", b, ti, d))])
                    k.dma("sp", S["G32"][b, ti, d], G32.t[:], G32, reads=[G32], writes=[k.dk(("G", b, ti, d))])


Prog.phase_gdn_a = phase_gdn_a


def phase_gdn_b(self, l):
    cfg, k, I, S = self.cfg, self.k, self.I, self.S
    NB, CTX, NTC, NT = cfg.NB, cfg.CTX, cfg.NTC, cfg.NT
    P = self.P
    pc = {"i": 0}

    def nextP():
        pc["i"] += 1
        return P[pc["i"] % 8]

    chains = []
    for b in range(NB):
        for d in range(2):
            if d == 0:
                order = list(range(NT))
            else:
                order = list(range(NTC - 1, -1, -1)) + list(range(NT - 1, NTC - 1, -1))
            ci = len(chains)
            ch = dict(b=b, d=d, order=order,
                      S=k.sb(f"gS{ci}", [128, 4, 128], F32), Sb=k.sb(f"gSb{ci}", [128, 4, 128], BF16),
                      g16=[k.sb(f"gg16_{ci}_{i}", [128, 4, 4, 128], BF16) for i in range(2)],
                      g32=[k.sb(f"gg32_{ci}_{i}", [128, 520], F32) for i in range(2)],
                      vn=k.sb(f"gvn{ci}", [128, 4, 128], BF16),
                      o=[k.sb(f"go{ci}_{i}", [128, 512], F32) for i in range(2)])
            k.op("pool", lambda e: e.memset(ch["S"].t[:], 0.0), writes=[ch["S"]])
            k.op("pool", lambda e: e.memset(ch["Sb"].t[:], 0.0), writes=[ch["Sb"]])
            chains.append(ch)
    for step in range(NT):
        for ch in chains:
            b, d = ch["b"], ch["d"]
            ti = ch["order"][step]
            G16, G32, O = ch["g16"][step % 2], ch["g32"][step % 2], ch["o"][step % 2]
            Sf, Sb, vn = ch["S"], ch["Sb"], ch["vn"]
            k.dma("sp", G16.t[:].rearrange("p a h x -> p (a h x)"), S["G16"][b, ti, d], G16, reads=[k.dk(("G", b, ti, d))], writes=[G16])
            k.dma("sp", G32.t[:], S["G32"][b, ti, d], G32, reads=[k.dk(("G", b, ti, d))], writes=[G32])
            for c in ((0, 1) if d == 0 else (1, 0)):
                cs = slice(c * 64, (c + 1) * 64)
                pv = nextP()
                for h in range(4):
                    k.op("pe", lambda e: e.matmul(pv.t[cs, h * 128:(h + 1) * 128], G16.t[:, 0, h, cs], Sb.t[:, h, :], start=True, stop=True), reads=[G16, Sb], writes=[pv])
                k.op("dve", lambda e: e.tensor_tensor(out=vn.t[cs].rearrange("p h x -> p (h x)"), in0=pv.t[cs, :], in1=G32.t[cs, 0:512], op=ALU.add), reads=[pv, G32], writes=[vn])
                po = nextP()
                for h in range(4):
                    k.op("pe", lambda e: e.matmul(po.t[cs, h * 128:(h + 1) * 128], G16.t[:, 2, h, cs], Sb.t[:, h, :], start=True, stop=False), reads=[G16, Sb], writes=[po])
                    k.op("pe", lambda e: e.matmul(po.t[cs, h * 128:(h + 1) * 128], G16.t[cs, 1, h, cs], vn.t[cs, h, :], start=False, stop=True), reads=[G16, vn], writes=[po])
                k.op("act", lambda e: e.activation(out=O.t[cs, :], in_=po.t[cs, :], func=AF.Copy), reads=[po], writes=[O])
                pS = nextP()
                for h in range(4):
                    k.op("pe", lambda e: e.matmul(pS.t[:, h * 128:(h + 1) * 128], G16.t[cs, 3, h, :], vn.t[cs, h, :], start=True, stop=True), reads=[G16, vn], writes=[pS])
                for h in range(4):
                    k.op("dve", lambda e: e.scalar_tensor_tensor(out=Sf.t[:, h, :], in0=Sf.t[:, h, :], scalar=G32.t[:, 512 + c * 4 + h:512 + c * 4 + h + 1],
                                                                 in1=pS.t[:, h * 128:(h + 1) * 128], op0=ALU.mult, op1=ALU.add), reads=[G32, pS], writes=[Sf])
                k.op("act", lambda e: e.activation(out=Sb.t[:], in_=Sf.t[:], func=AF.Copy), reads=[Sf], writes=[Sb])
            k.dma("sp", S["ODN"][b, d, ti * 128:(ti + 1) * 128, :], O.t[:], O, reads=[O], writes=[k.dk(("ODN", b, d, ti))])


Prog.phase_gdn_b = phase_gdn_b


def bcast_load(self, name, src_row_ap, n):
    k = self.k
    t = k.sb(name, [128, n], F32)
    k.dma("sp", t.t[:], src_row_ap.broadcast_to([128, n]), t, reads=[k.dk(("GROW", 0)), k.dk(("GROW", 1)), k.dk(("GROW", 2)), k.dk(("GROW", 3))], writes=[t])
    return t


def resid_ln(self, xt, ysrc, ybufs, GB, LNG, LNB, r, small, outb):
    k, cfg = self.k, self.cfg
    for half in range(2):
        hs = slice(half * 512, (half + 1) * 512)
        k.op("dve", lambda e: e.tensor_tensor(out=r.t[:, hs], in0=ysrc[half], in1=GB.t[:, hs], op=ALU.mult), reads=[ybufs[half], GB], writes=[r])
    k.op("dve", lambda e: e.scalar_tensor_tensor(out=r.t[:], in0=xt.t[:], scalar=float(cfg.ALPHA), in1=r.t[:], op0=ALU.mult, op1=ALU.add), reads=[xt, r], writes=[r])
    st = small.t[:, 0:12].rearrange("p (a s) -> p a s", a=2)
    for half in range(2):
        k.op("dve", lambda e: e.bn_stats(out=st[:, half, :], in_=r.t[:, half * 512:(half + 1) * 512]), reads=[r], writes=[small])
    k.op("dve", lambda e: e.bn_aggr(out=small.t[:, 12:14], in_=small.t[:, 0:12]), reads=[small], writes=[small])
    k.op("dve", lambda e: e.tensor_scalar(out=small.t[:, 14:15], in0=small.t[:, 13:14], scalar1=1e-5, scalar2=None, op0=ALU.add), reads=[small], writes=[small])
    k.op("act", lambda e: e.activation(out=small.t[:, 14:15], in_=small.t[:, 14:15], func=AF.Sqrt), reads=[small], writes=[small])
    k.op("dve", lambda e: e.reciprocal(out=small.t[:, 15:16], in_=small.t[:, 14:15]), reads=[small], writes=[small])
    k.op("dve", lambda e: e.tensor_scalar(out=r.t[:], in0=r.t[:], scalar1=small.t[:, 12:13], scalar2=small.t[:, 15:16], op0=ALU.subtract, op1=ALU.mult), reads=[r, small], writes=[r])
    k.op("pool", lambda e: e.tensor_tensor(out=r.t[:], in0=r.t[:], in1=LNG.t[:], op=ALU.mult), reads=[r, LNG], writes=[r])
    k.op("pool", lambda e: e.tensor_tensor(out=outb.t[:], in0=r.t[:], in1=LNB.t[:], op=ALU.add), reads=[r, LNB], writes=[outb])


def phase_merge(self, l):
    cfg, k, I, S = self.cfg, self.k, self.I, self.S
    NB, CTX, NTC, NT = cfg.NB, cfg.CTX, cfg.NTC, cfg.NT
    P = self.P
    pc = {"i": 0}

    def nextP():
        pc["i"] += 1
        return P[pc["i"] % 8]

    NAG = bcast_load(self, "nag", I["na_out_g"][l:l + 1, :], 512)
    DNG = bcast_load(self, "dng", I["dn_norm_g"][l:l + 1, :], 128)
    G1 = [bcast_load(self, f"g1b{v}", S["GROW"][l, 0, v:v + 1, :], 1024) for v in range(NB + 1)]
    LNG = bcast_load(self, "ln1g", I["ln1_g"][l:l + 1, :], 1024)
    LNB = bcast_load(self, "ln1b", I["ln1_b"][l:l + 1, :], 1024)
    wout = k.sb("wout", [128, 8, 1024], BF16)
    k.dma("pool", wout.t[:], I["w_out"][l].rearrange("(kc p) n -> p kc n", p=128), wout, writes=[wout])
    NS = 2
    ona = [k.sb(f"m_ona{i}", [128, 512], F32) for i in range(NS)]
    odf = [k.sb(f"m_odf{i}", [128, 512], F32) for i in range(NS)]
    odb = [k.sb(f"m_odb{i}", [128, 512], F32) for i in range(NS)]
    zs = [k.sb(f"m_zs{i}", [128, 512], BF16) for i in range(NS)]
    xt = [k.sb(f"m_x{i}", [128, 1024], F32) for i in range(NS)]
    sq = k.sb("m_sq", [128, 512], F32)
    tmp = k.sb("m_tmp", [128, 512], F32)
    st = [k.sb(f"m_st{i}", [128, 8], F32) for i in range(NS)]
    mg = [k.sb(f"m_mg{i}", [128, 1024], BF16) for i in range(NS)]
    mT = [k.sb(f"m_mT{i}", [128, 8, 128], BF16) for i in range(NS)]
    r = [k.sb(f"m_r{i}", [128, 1024], F32) for i in range(NS)]
    sm = [k.sb(f"m_sm{i}", [128, 16], F32) for i in range(NS)]
    ob = [k.sb(f"m_ob{i}", [128, 1024], F32) for i in range(NS)]
    src = I["x0"] if l == 0 else S["X"]
    it = 0
    for b in range(NB):
        for ti in range(NT):
            u = it % NS
            it += 1
            v = NB if ti < NTC else b
            rows = slice(ti * 128, (ti + 1) * 128)
            ona_keys = [k.dk(("ONA", b, h, R)) for h in range(8) for R in range(-1, cfg.ROWS // 8)]
            k.dma("sp", ona[u].t[:], S["ONA"][b, rows, :], ona[u], reads=ona_keys, writes=[ona[u]])
            k.dma("sp", odf[u].t[:], S["ODN"][b, 0, rows, :], odf[u], reads=[k.dk(("ODN", b, 0, ti))], writes=[odf[u]])
            k.dma("sp", odb[u].t[:], S["ODN"][b, 1, rows, :], odb[u], reads=[k.dk(("ODN", b, 1, ti))], writes=[odb[u]])
            k.dma("sp", zs[u].t[:], S["ZS"][b, rows, :], zs[u], reads=[k.dk(("ZS", b, ti * 128))], writes=[zs[u]])
            k.dma("sp", xt[u].t[:], src[b, rows, :], xt[u], reads=[k.dk(("X", b, ti))], writes=[xt[u]])
            O, F, B_, Z, ST = ona[u], odf[u], odb[u], zs[u], st[u]
            k.op("pool", lambda e: e.tensor_tensor(out=F.t[:], in0=F.t[:], in1=B_.t[:], op=ALU.add), reads=[F, B_], writes=[F])
            k.op("act", lambda e: e.activation(out=sq.t[:], in_=O.t[:], func=AF.Square, accum_out=ST.t[:, 0:1]), reads=[O], writes=[sq, ST])
            k.op("dve", lambda e: e.tensor_tensor(out=tmp.t[:], in0=F.t[:], in1=F.t[:], op=ALU.mult), reads=[F], writes=[tmp])
            k.op("dve", lambda e: e.tensor_reduce(out=ST.t[:, 1:5], in_=tmp.t[:].rearrange("p (h x) -> p h x", x=128), axis=AX.X, op=ALU.add), reads=[tmp], writes=[ST])
            k.op("dve", lambda e: e.tensor_scalar(out=ST.t[:, 0:1], in0=ST.t[:, 0:1], scalar1=1.0 / 512, scalar2=1e-6, op0=ALU.mult, op1=ALU.add), reads=[ST], writes=[ST])
            k.op("dve", lambda e: e.tensor_scalar(out=ST.t[:, 1:5], in0=ST.t[:, 1:5], scalar1=1.0 / 128, scalar2=1e-6, op0=ALU.mult, op1=ALU.add), reads=[ST], writes=[ST])
            k.op("act", lambda e: e.activation(out=ST.t[:, 0:5], in_=ST.t[:, 0:5], func=AF.Sqrt), reads=[ST], writes=[ST])
            k.op("dve", lambda e: e.reciprocal(out=ST.t[:, 0:5], in_=ST.t[:, 0:5]), reads=[ST], writes=[ST])
            M = mg[u]
            k.op("dve", lambda e: e.scalar_tensor_tensor(out=M.t[:, 0:512], in0=O.t[:], scalar=ST.t[:, 0:1], in1=NAG.t[:], op0=ALU.mult, op1=ALU.mult), reads=[O, ST, NAG], writes=[M])
            k.op("dve", lambda e: e.tensor_tensor(out=tmp.t[:].rearrange("p (h x) -> p h x", x=128), in0=F.t[:].rearrange("p (h x) -> p h x", x=128),
                                                  in1=ST.t[:, 1:5].unsqueeze(2).broadcast_to([128, 4, 128]), op=ALU.mult), reads=[F, ST], writes=[tmp])
            k.op("pool", lambda e: e.tensor_tensor(out=tmp.t[:].rearrange("p (h x) -> p h x", x=128), in0=tmp.t[:].rearrange("p (h x) -> p h x", x=128),
                                                   in1=DNG.t[:].unsqueeze(1).broadcast_to([128, 4, 128]), op=ALU.mult), reads=[tmp, DNG], writes=[tmp])
            k.op("dve", lambda e: e.tensor_tensor(out=M.t[:, 512:1024], in0=tmp.t[:], in1=Z.t[:], op=ALU.mult), reads=[tmp, Z], writes=[M])
            pt = nextP()
            ptb = pt.t[:].bitcast(BF16)
            for kc in range(8):
                k.op("pe", lambda e: e.transpose(out=ptb[:, kc * 128:(kc + 1) * 128], in_=M.t[:, kc * 128:(kc + 1) * 128], identity=self.identB.t[:]), reads=[M, self.identB], writes=[pt])
            k.op("act", lambda e: e.activation(out=mT[u].t[:], in_=ptb.rearrange("p (c x) -> p c x", x=128), func=AF.Copy), reads=[pt], writes=[mT[u]])
            py = [nextP(), nextP()]
            for half in range(2):
                for kc in range(8):
                    k.op("pe", lambda e: e.matmul(py[half].t[:, :], mT[u].t[:, kc, :], wout.t[:, kc, half * 512:(half + 1) * 512], start=(kc == 0), stop=(kc == 7)),
                         reads=[mT[u], wout], writes=[py[half]])
            resid_ln(self, xt[u], [py[0].t[:, :], py[1].t[:, :]], py, G1[v], LNG, LNB, r[u], sm[u], ob[u])
            k.dma("sp", S["X"][b, rows, :], ob[u].t[:], ob[u], reads=[ob[u]], writes=[k.dk(("X", b, ti))])


Prog.phase_merge = phase_merge


def phase_ffn(self, l):
    cfg, k, I, S = self.cfg, self.k, self.I, self.S
    NB, CTX, NTC, NT, L = cfg.NB, cfg.CTX, cfg.NTC, cfg.NT, cfg.DEPTH
    P = self.P
    pc = {"i": 0}

    def nextP():
        pc["i"] += 1
        return P[pc["i"] % 8]

    moe = (l % 2 == 1)
    li = l // 2
    FF = cfg.FFE if moe else cfg.FFD
    NE = cfg.NEXP if moe else 1
    fbs = []
    f0 = 0
    while f0 < FF:
        n = min(512, FF - f0)
        fbs.append((f0, n))
        f0 += n
    G2 = [bcast_load(self, f"g2b{v}", S["GROW"][l, 1, v:v + 1, :], 1024) for v in range(NB + 1)]
    LNG = bcast_load(self, "ln2g", I["ln2_g"][l:l + 1, :], 1024)
    LNB = bcast_load(self, "ln2b", I["ln2_b"][l:l + 1, :], 1024)
    GT = 8
    xg = k.sb("f_xg", [128, GT, 1024], F32)
    hT = k.sb("f_hT", [128, 8, GT * 128], BF16)
    hTf = [k.sb(f"f_hTf{i}", [128, 8, 128], F32) for i in range(2)]
    yg = k.sb("f_yg", [128, GT, 1024], F32)
    wg = [k.sb(f"f_wg{i}", [128, 8, 512], BF16) for i in range(2)]
    wu = [k.sb(f"f_wu{i}", [128, 8, 512], BF16) for i in range(2)]
    wd = [k.sb(f"f_wd{i}", [128, 4, 1024], BF16) for i in range(2)]
    hid = [k.sb(f"f_hid{i}", [128, 4, GT * 128], BF16) for i in range(1)]
    sg = [k.sb(f"f_sg{i}", [128, 512], BF16) for i in range(2)]
    tmp = [k.sb(f"f_tmp{i}", [128, 512], F32) for i in range(2)]
    r = [k.sb(f"f_r{i}", [128, 1024], F32) for i in range(1)]
    sm = [k.sb(f"f_sm{i}", [128, 16], F32) for i in range(2)]
    ob = [k.sb(f"f_ob{i}", [128, 1024], F32) for i in range(2)]
    if moe:
        rw = k.sb("f_rw", [128, 8, 8], F32)
        k.dma("sp", rw.t[:], I["moe_router"][li].rearrange("(kc p) e -> p kc e", p=128), rw, writes=[rw])
        gates = k.sb("f_gates", [128, GT, 8], F32)
        gs = [k.sb(f"f_gs{i}", [128, 4, 8], F32) for i in range(2)]
    tiles = [(b, ti) for b in range(NB) for ti in range(NT)]
    cnt = {"w": 0, "s": 0, "t": 0, "o": 0}
    last = (l == L - 1)
    for g0 in range(0, len(tiles), GT):
        gt = tiles[g0:g0 + GT]
        ng = len(gt)
        k.op("pool", lambda e: e.memset(yg.t[:, 0:ng, :], 0.0), writes=[yg])
        for j, (b, ti) in enumerate(gt):
            v = NB if ti < NTC else b
            k.dma("sp", xg.t[:, j, :], S["X"][b, ti * 128:(ti + 1) * 128, :], xg, reads=[k.dk(("X", b, ti))], writes=[xg])
            hf = hTf[j % 2]
            for q in range(2):
                pb = nextP()
                for c4 in range(4):
                    kc = q * 4 + c4
                    k.op("pe", lambda e: e.transpose(out=pb.t[:, c4 * 128:(c4 + 1) * 128], in_=xg.t[:, j, kc * 128:(kc + 1) * 128], identity=self.identF.t[:]), reads=[xg, self.identF], writes=[pb])
                for c4 in range(4):
                    kc = q * 4 + c4
                    k.op("act", lambda e: e.activation(out=hf.t[:, kc, :], in_=pb.t[:, c4 * 128:(c4 + 1) * 128], func=AF.Identity,
                                                       scale=self.modT.t[:, l, 32 + kc, v:v + 1], bias=self.modT.t[:, l, 24 + kc, v:v + 1]), reads=[pb, self.modT], writes=[hf])
            k.op("dve", lambda e: e.tensor_copy(out=hT.t[:, :, j * 128:(j + 1) * 128], in_=hf.t[:]), reads=[hf], writes=[hT])
            if moe:
                pl = nextP()
                for kc in range(8):
                    k.op("pe", lambda e: e.matmul(pl.t[:, 0:8], hf.t[:, kc, :], rw.t[:, kc, :], start=(kc == 0), stop=(kc == 7)), reads=[hf, rw], writes=[pl])
                GS = gs[j % 2]
                lg, t8, mk_, ex_ = GS.t[:, 0, :], GS.t[:, 1, :], GS.t[:, 2, :], GS.t[:, 3, :]
                k.op("dve", lambda e: e.tensor_copy(out=lg, in_=pl.t[:, 0:8]), reads=[pl], writes=[GS])
                k.op("dve", lambda e: e.max(out=t8, in_=lg), reads=[GS], writes=[GS])
                k.op("dve", lambda e: e.tensor_scalar(out=mk_, in0=lg, scalar1=GS.t[:, 1, 1:2], scalar2=None, op0=ALU.is_ge), reads=[GS], writes=[GS])
                k.op("dve", lambda e: e.tensor_scalar(out=ex_, in0=lg, scalar1=GS.t[:, 1, 0:1], scalar2=None, op0=ALU.subtract), reads=[GS], writes=[GS])
                k.op("act", lambda e: e.activation(out=ex_, in_=ex_, func=AF.Exp), reads=[GS], writes=[GS])
                k.op("dve", lambda e: e.tensor_tensor(out=ex_, in0=ex_, in1=mk_, op=ALU.mult), reads=[GS], writes=[GS])
                k.op("dve", lambda e: e.tensor_reduce(out=GS.t[:, 1, 2:3], in_=ex_, axis=AX.X, op=ALU.add), reads=[GS], writes=[GS])
                k.op("dve", lambda e: e.reciprocal(out=GS.t[:, 1, 3:4], in_=GS.t[:, 1, 2:3]), reads=[GS], writes=[GS])
                k.op("dve", lambda e: e.tensor_scalar(out=gates.t[:, j, :], in0=ex_, scalar1=GS.t[:, 1, 3:4], scalar2=None, op0=ALU.mult), reads=[GS], writes=[gates])
        ntok = ng * 128
        sbs = [(s0, min(512, ntok - s0)) for s0 in range(0, ntok, 512)]
        for e_ in range(NE):
            if moe:
                Wg, Wu, Wd = I["moe_w_gate"][li, e_], I["moe_w_up"][li, e_], I["moe_w_down"][li, e_]
            else:
                Wg, Wu, Wd = I["ffn_w_gate"][li], I["ffn_w_up"][li], I["ffn_w_down"][li]
            for (f0, fn) in fbs:
                wi = cnt["w"] % 2
                cnt["w"] += 1
                nfc = fn // 128
                k.dma("pool", wg[wi].t[:, :, 0:fn], Wg[:, f0:f0 + fn].rearrange("(kc p) n -> p kc n", p=128), wg[wi], writes=[wg[wi]])
                k.dma("pool", wu[wi].t[:, :, 0:fn], Wu[:, f0:f0 + fn].rearrange("(kc p) n -> p kc n", p=128), wu[wi], writes=[wu[wi]])
                k.dma("pool", wd[wi].t[:, 0:nfc, :], Wd[f0:f0 + fn, :].rearrange("(c p) n -> p c n", p=128), wd[wi], writes=[wd[wi]])
                H = hid[0]
                for (s0, sn) in sbs:
                    for fc in range(nfc):
                        pg_, pu_ = nextP(), nextP()
                        for kc in range(8):
                            k.op("pe", lambda e: e.matmul(pg_.t[:, 0:sn], wg[wi].t[:, kc, fc * 128:(fc + 1) * 128], hT.t[:, kc, s0:s0 + sn], start=(kc == 0), stop=(kc == 7)), reads=[wg[wi], hT], writes=[pg_])
                        for kc in range(8):
                            k.op("pe", lambda e: e.matmul(pu_.t[:, 0:sn], wu[wi].t[:, kc, fc * 128:(fc + 1) * 128], hT.t[:, kc, s0:s0 + sn], start=(kc == 0), stop=(kc == 7)), reads=[wu[wi], hT], writes=[pu_])
                        SG = sg[cnt["s"] % 2]
                        cnt["s"] += 1
                        k.op("act", lambda e: e.activation(out=SG.t[:, 0:sn], in_=pg_.t[:, 0:sn], func=AF.Silu), reads=[pg_], writes=[SG])
                        k.op("dve", lambda e: e.tensor_tensor(out=H.t[:, fc, s0:s0 + sn], in0=pu_.t[:, 0:sn], in1=SG.t[:, 0:sn], op=ALU.mult), reads=[pu_, SG], writes=[H])
                for j in range(ng):
                    for half in range(2):
                        pd_ = nextP()
                        for fc in range(nfc):
                            k.op("pe", lambda e: e.matmul(pd_.t[:, :], H.t[:, fc, j * 128:(j + 1) * 128], wd[wi].t[:, fc, half * 512:(half + 1) * 512], start=(fc == 0), stop=(fc == nfc - 1)),
                                 reads=[H, wd[wi]], writes=[pd_])
                        T_ = tmp[cnt["t"] % 2]
                        cnt["t"] += 1
                        if moe:
                            k.op("act", lambda e: e.activation(out=T_.t[:], in_=pd_.t[:, :], func=AF.Copy, scale=gates.t[:, j, e_:e_ + 1]), reads=[pd_, gates], writes=[T_])
                        else:
                            k.op("act", lambda e: e.activation(out=T_.t[:], in_=pd_.t[:, :], func=AF.Copy), reads=[pd_], writes=[T_])
                        k.op("pool", lambda e: e.tensor_tensor(out=yg.t[:, j, half * 512:(half + 1) * 512], in0=yg.t[:, j, half * 512:(half + 1) * 512], in1=T_.t[:], op=ALU.add), reads=[T_, yg], writes=[yg])
        for j, (b, ti) in enumerate(gt):
            v = NB if ti < NTC else b
            o_ = ob[cnt["o"] % 2]
            s_ = sm[cnt["o"] % 2]
            cnt["o"] += 1
            resid_ln_x(self, xg, j, yg, G2[v], LNG, LNB, r[0], s_, o_)
            if last:
                if ti >= NTC:
                    k.dma("sp", self.out[b, (ti - NTC) * 128:(ti - NTC + 1) * 128, :], o_.t[:], o_, reads=[o_], writes=[k.dk(("Y", b, ti))])
            else:
                k.dma("sp", S["X"][b, ti * 128:(ti + 1) * 128, :], o_.t[:], o_, reads=[o_], writes=[k.dk(("X", b, ti))])


def resid_ln_x(self, xg, j, yg, GB, LNG, LNB, r, small, outb):
    class V:
        pass
    xv = V()
    xv.t = xg.t[:, j, :]
    class A:
        def __init__(s, ap):
            s.ap = ap
        def __getitem__(s, idx):
            return s.ap
    xa = Buf(A(xg.t[:, j, :]), "xa")
    xa.w, xa.r = xg.w, xg.r
    resid_ln(self, xa, [yg.t[:, j, 0:512], yg.t[:, j, 512:1024]], [yg, yg], GB, LNG, LNB, r, small, outb)
    for s_, v_ in xa.r.items():
        if xg.r.get(s_, 0) < v_:
            xg.r[s_] = v_


Prog.phase_ffn = phase_ffn


_PROG = {}


def kernel(**inputs):
    cfg = Cfg()
    inp = {k_: np.asarray(v) for k_, v in inputs.items()}
    if "prog" not in _PROG:
        _PROG["prog"] = Prog(cfg)
    prog = _PROG["prog"]
    shared = shared_inputs(cfg, inp)
    names = set(prog.I.keys())
    in_maps = []
    for core in range(cfg.NCORES):
        ci = prep_core_inputs(cfg, inp, core)
        ci.update(shared)
        in_maps.append({k_: v for k_, v in ci.items() if k_ in names})
    res = run_bass_kernel_spmd(prog.k.nc, in_maps, core_ids=list(range(cfg.NCORES)))
    out = np.concatenate([r["y"] for r in res.results], axis=0)
    return out.astype(np.float32)
```

```python
import numpy as np
from contextlib import ExitStack
import concourse.bass as bass
import concourse.mybir as mybir
from concourse.bass_utils import run_bass_kernel_spmd

F32 = mybir.dt.float32
BF16 = mybir.dt.bfloat16
AF = mybir.ActivationFunctionType
ALU = mybir.AluOpType
AX = mybir.AxisListType


class Buf:
    __slots__ = ("t", "name", "w", "r", "dsem", "dcnt")

    def __init__(self, t, name):
        self.t = t
        self.name = name
        self.w = None
        self.r = {}
        self.dsem = None
        self.dcnt = 0


class K:
    def __init__(self, debug_outs=()):
        self.nc = bass.Bass("TRN2", target_bir_lowering=False)
        self.es = ExitStack()
        nc = self.nc
        self.E = {"pe": nc.tensor, "act": nc.scalar, "dve": nc.vector, "pool": nc.gpsimd, "sp": nc.sync}
        self.sems = []
        self.esem = {}
        self.ecnt = {}
        for e in self.E:
            self.esem[e] = self.newsem("e_" + e)
            self.ecnt[e] = 0
        self.waited = {e: {} for e in self.E}
        self.dkeys = {}
        self.debug_outs = set(debug_outs)
        self.ninstr = 0
        self.dma_bufs = []
        self.stacks = [self.es]
        self.scope_bufs = [[]]
        self.free_sems = []
        self.semcount = {}

    def phase_begin(self):
        st = ExitStack()
        self.stacks.append(st)
        self.scope_bufs.append([])

    def phase_end(self):
        self.barrier_all()
        for b in self.scope_bufs.pop():
            if b.dsem is not None:
                self.semcount[b.dsem] = b.dcnt
                self.free_sems.append(b.dsem)
                self.dma_bufs.remove(b)
        self.stacks.pop().close()

    def barrier_all(self):
        need = {}
        for e in self.E:
            if self.ecnt[e]:
                need[self.esem[e]] = self.ecnt[e]
        for b in self.dma_bufs:
            need[b.dsem] = b.dcnt
        for e in self.E:
            self._wait(e, (dict(need), {}), is_dma=True)

    def newsem(self, name):
        s = self.es.enter_context(self.nc.semaphore(name))
        self.sems.append(s)
        return len(self.sems) - 1

    def sb(self, name, shape, dt):
        self.nalloc = getattr(self, "nalloc", 0) + 1
        t = self.stacks[-1].enter_context(self.nc.sbuf_tensor("s%d_" % self.nalloc + name, list(shape), dt))
        b = Buf(t, name)
        self.scope_bufs[-1].append(b)
        return b

    def ps(self, name, shape, dt):
        t = self.es.enter_context(self.nc.psum_tensor("p_" + name, list(shape), dt))
        return Buf(t, name)

    def dram_in(self, name, shape, dt):
        return self.nc.dram_tensor(name, list(shape), dt, kind="ExternalInput").ap()

    def dram_out(self, name, shape, dt):
        return self.nc.dram_tensor(name, list(shape), dt, kind="ExternalOutput").ap()

    def dram_scr(self, name, shape, dt):
        kind = "ExternalOutput" if name in self.debug_outs else "Internal"
        return self.nc.dram_tensor(name, list(shape), dt, kind=kind).ap()

    def dk(self, key):
        b = self.dkeys.get(key)
        if b is None:
            b = Buf(None, str(key))
            self.dkeys[key] = b
        return b

    def _need(self, reads, writes):
        need = {}
        hard = {}

        def add(d, tok):
            if tok is None:
                return
            s, v = tok
            if d.get(s, 0) < v:
                d[s] = v

        for b in reads:
            add(need, b.w)
            add(hard, b.w)
        for b in writes:
            add(need, b.w)
            for s, v in b.r.items():
                add(need, (s, v))
        return need, hard

    def _wait(self, e, nh, is_dma=False):
        need, hard = nh
        own = self.esem[e]
        wd = self.waited[e]
        for s, v in need.items():
            if s == own and not is_dma:
                if e == "pe" or hard.get(s, 0) < v:
                    v = hard.get(s, 0)
                    if e == "pe" or v == 0:
                        continue
            if wd.get(s, 0) >= v:
                continue
            self.E[e].wait_ge(self.sems[s], v)
            wd[s] = v
            self.ninstr += 1

    def _commit(self, tok, reads, writes):
        s, v = tok
        for b in reads:
            if b.r.get(s, 0) < v:
                b.r[s] = v
        for b in writes:
            b.w = tok
            b.r = {}

    def op(self, e, fn, reads=(), writes=()):
        self._wait(e, self._need(reads, writes))
        ins = fn(self.E[e])
        self.ecnt[e] += 1
        ins.then_inc(self.sems[self.esem[e]], 1)
        self.ninstr += 1
        self._commit((self.esem[e], self.ecnt[e]), reads, writes)

    def dma(self, q, out, in_, sbuf, reads=(), writes=(), **kw):
        if sbuf.dsem is None:
            if self.free_sems:
                sbuf.dsem = self.free_sems.pop()
                sbuf.dcnt = self.semcount[sbuf.dsem]
            else:
                sbuf.dsem = self.newsem("d%d_" % len(self.sems) + sbuf.name)
            self.dma_bufs.append(sbuf)
        self._wait(q, self._need(reads, writes), is_dma=True)
        sbuf.dcnt += 16
        self.E[q].dma_start(out=out, in_=in_, **kw).then_inc(self.sems[sbuf.dsem], 16)
        self.ninstr += 1
        self._commit((sbuf.dsem, sbuf.dcnt), reads, writes)

    def finish(self):
        need = {}
        for e in self.E:
            if self.ecnt[e]:
                need[self.esem[e]] = self.ecnt[e]
        for b in self.dma_bufs:
            need[b.dsem] = b.dcnt
        self._wait("sp", (need, {}), is_dma=True)
        while len(self.stacks) > 1:
            self.stacks.pop().close()
        self.es.close()


class Cfg:
    def __init__(s, NB=2, ROWS=64, CTX=256, DEPTH=4, FFD=2816, FFE=3584, NEXP=8, NCORES=8):
        s.NB, s.ROWS, s.CTX, s.DEPTH, s.FFD, s.FFE, s.NEXP, s.NCORES = NB, ROWS, CTX, DEPTH, FFD, FFE, NEXP, NCORES
        s.D = 1024
        s.GW = 64
        s.SEQ = ROWS * 64
        s.TOK = CTX + s.SEQ
        s.NT = s.TOK // 128
        s.NTC = CTX // 128
        s.ALPHA = (2.0 * DEPTH) ** 0.25
        s.PIN = 3600
        s.NV = 4
        assert NB + 1 <= 4

    def groups(s):
        g = []
        t = 0
        while t < s.CTX:
            n = min(512, s.CTX - t)
            g.append((t, n, True))
            t += n
        while t < s.TOK:
            n = min(512, s.TOK - t)
            g.append((t, n, False))
            t += n
        return g


def host_consts(cfg):
    import ml_dtypes
    c = {}
    c["identF"] = np.eye(128, dtype=np.float32)
    return c


def prep_core_inputs(cfg, inp, core):
    NB = cfg.NB
    b0 = core * NB
    L = cfg.DEPTH
    d = {}
    x0 = np.concatenate([inp["ctx"][b0:b0 + NB], inp["x"][b0:b0 + NB]], axis=1)
    d["x0"] = np.ascontiguousarray(x0, dtype=np.float32)
    cc = np.zeros((4, 1024), np.float32)
    cc[:NB] = inp["c"][b0:b0 + NB]
    cc[NB] = inp["c_ctx"]
    d["cT"] = np.ascontiguousarray(cc.reshape(4, 8, 128).transpose(2, 1, 0))
    d["w_mod"] = inp["w_mod"]
    d["bT"] = np.ascontiguousarray(inp["b_mod"].reshape(L, 48, 128).transpose(2, 0, 1))
    d["w_in"] = inp["w_in"]
    return d


def shared_inputs(cfg, inp):
    d = {}
    d.update(host_consts(cfg))
    d["BT"] = na_tables(cfg, inp["na_rpb"])
    d.update(gdn_consts(cfg))
    L = cfg.DEPTH
    d["cwT"] = np.ascontiguousarray(inp["dn_conv_w"].reshape(L, 5, 12, 128).transpose(3, 0, 2, 1))
    d["a_log"] = np.ascontiguousarray(inp["dn_a_log"].reshape(L, 8))
    for nm_ in ("na_out_g", "dn_norm_g", "w_out", "ln1_g", "ln1_b", "ln2_g", "ln2_b", "ffn_w_gate", "ffn_w_up", "ffn_w_down", "moe_router", "moe_w_gate", "moe_w_up", "moe_w_down"):
        d[nm_] = inp[nm_]
    d["dt_bias"] = np.ascontiguousarray(inp["dn_dt_bias"].reshape(L, 8))
    return d


class Prog:
    def __init__(self, cfg, debug_outs=(), stop_after=None):
        self.cfg = cfg
        self.k = K(debug_outs)
        self.stop_after = stop_after
        self.build()

    def build(self):
        cfg, k = self.cfg, self.k
        L, NB, TOK = cfg.DEPTH, cfg.NB, cfg.TOK
        I = self.I = {}
        I["x0"] = k.dram_in("x0", [NB, TOK, 1024], F32)
        I["cT"] = k.dram_in("cT", [128, 8, 4], F32)
        I["w_mod"] = k.dram_in("w_mod", [L, 1024, 6144], F32)
        I["bT"] = k.dram_in("bT", [128, L, 48], F32)
        I["w_in"] = k.dram_in("w_in", [L, 1024, 3600], F32)
        I["identF"] = k.dram_in("identF", [128, 128], F32)
        S = self.S = {}
        S["GROW"] = k.dram_scr("GROW", [L, 2, 4, 1024], F32)
        S["FT"] = k.dram_scr("FT", [NB, 2560, TOK], BF16)
        S["VA"] = k.dram_scr("VA", [NB, TOK, 8, 65], BF16)
        S["ZS"] = k.dram_scr("ZS", [NB, TOK, 512], BF16)
        S["BA"] = k.dram_scr("BA", [NB, TOK, 16], F32)
        S["ONA"] = k.dram_scr("ONA", [NB, TOK, 512], F32)
        S["X"] = k.dram_scr("X", [NB, TOK, 1024], F32)
        S["G16"] = k.dram_scr("G16", [NB, cfg.NT, 2, 128, 2048], BF16)
        S["G32"] = k.dram_scr("G32", [NB, cfg.NT, 2, 128, 520], F32)
        S["ODN"] = k.dram_scr("ODN", [NB, 2, TOK, 512], F32)
        I["gmask"] = k.dram_in("gmask", [128, 8, 128], F32)
        I["gmb"] = k.dram_in("gmb", [128, 4, 128], F32)
        I["ropec"] = k.dram_in("ropec", [128, cfg.NT - cfg.NTC, 64], F32)
        I["ropes"] = k.dram_in("ropes", [128, cfg.NT - cfg.NTC, 64], F32)
        I["cwT"] = k.dram_in("cwT", [128, L, 12, 5], F32)
        I["a_log"] = k.dram_in("a_log", [L, 8], F32)
        for nm_, sh in (("na_out_g", [L, 512]), ("dn_norm_g", [L, 128]), ("w_out", [L, 1024, 1024]), ("ln1_g", [L, 1024]), ("ln1_b", [L, 1024]),
                        ("ln2_g", [L, 1024]), ("ln2_b", [L, 1024]), ("ffn_w_gate", [(L + 1) // 2, 1024, cfg.FFD]), ("ffn_w_up", [(L + 1) // 2, 1024, cfg.FFD]),
                        ("ffn_w_down", [(L + 1) // 2, cfg.FFD, 1024]), ("moe_router", [L // 2, 1024, 8]), ("moe_w_gate", [L // 2, 8, 1024, cfg.FFE]),
                        ("moe_w_up", [L // 2, 8, 1024, cfg.FFE]), ("moe_w_down", [L // 2, 8, cfg.FFE, 1024])):
            I[nm_] = k.dram_in(nm_, sh, F32)
        I["dt_bias"] = k.dram_in("dt_bias", [L, 8], F32)
        self.NPL = len(na_plan(cfg)[1])
        I["BT"] = k.dram_in("BT", [L, 8, 64, self.NPL, 8, 64], F32)
        self.out = k.dram_out("y", [NB, cfg.SEQ, 1024], F32)
        self.P = [k.ps(f"pb{i}", [128, 512], F32) for i in range(8)]
        self.identF = k.sb("identF", [128, 128], F32)
        k.dma("sp", self.identF.t[:], I["identF"][:, :], self.identF, writes=[self.identF])
        self.identB = k.sb("identB", [128, 128], BF16)
        k.op("dve", lambda e: e.tensor_copy(out=self.identB.t[:], in_=self.identF.t[:]), reads=[self.identF], writes=[self.identB])
        self.modT = k.sb("modT", [128, L, 48, 4], F32)
        k.phase_begin()
        self.phase_mod()
        k.phase_end()
        if self.stop_after == "mod":
            return self.end_debug()
        for l in range(L):
            k.phase_begin()
            self.phase_proj(l)
            k.phase_end()
            if self.stop_after == ("proj", l):
                return self.end_debug()
            k.phase_begin()
            self.phase_na(l)
            k.phase_end()
            if self.stop_after == ("na", l):
                return self.end_debug()
            k.phase_begin()
            self.phase_gdn_a(l)
            k.phase_end()
            k.phase_begin()
            self.phase_gdn_b(l)
            k.phase_end()
            if self.stop_after == ("gdn", l):
                return self.end_debug()
            k.phase_begin()
            self.phase_merge(l)
            k.phase_end()
            if self.stop_after == ("merge", l):
                return self.end_debug()
            k.phase_begin()
            self.phase_ffn(l)
            k.phase_end()
            if self.stop_after == ("ffn", l):
                return self.end_debug()
        k.finish()

    def end_debug(self):
        self.k.finish()

    def phase_mod(self):
        cfg, k, I, S = self.cfg, self.k, self.I, self.S
        L = cfg.DEPTH
        cT = k.sb("cT", [128, 8, 4], F32)
        k.dma("sp", cT.t[:], I["cT"][:, :, :], cT, writes=[cT])
        sT = k.sb("sT", [128, 8, 4], F32)
        k.op("act", lambda e: e.activation(out=sT.t[:], in_=cT.t[:], func=AF.Silu), reads=[cT], writes=[sT])
        bT = k.sb("bT", [128, L, 48], F32)
        k.dma("sp", bT.t[:], I["bT"][:, :, :], bT, writes=[bT])
        modT = self.modT
        wbuf = [k.sb(f"wm{i}", [128, 8, 512], F32) for i in range(2)]
        grow = k.sb("grow", [4, 2048], F32)
        it = 0
        for l in range(L):
            for ng in range(12):
                wb = wbuf[it % 2]
                pm = self.P[it % 2]
                it += 1
                k.dma("sp", wb.t[:], I["w_mod"][l, :, ng * 512:(ng + 1) * 512].rearrange("(kc p) n -> p kc n", p=128), wb, writes=[wb])
                for c4 in range(4):
                    for kc in range(8):
                        k.op("pe", lambda e: e.matmul(pm.t[:, c4 * 4:(c4 + 1) * 4], wb.t[:, kc, c4 * 128:(c4 + 1) * 128], sT.t[:, kc, :],
                                                      start=(kc == 0), stop=(kc == 7)), reads=[wb, sT], writes=[pm])
                k.op("dve", lambda e: e.tensor_tensor(out=modT.t[:, l, ng * 4:(ng + 1) * 4, :],
                                                      in0=pm.t[:, 0:16].rearrange("p (c v) -> p c v", v=4),
                                                      in1=bT.t[:, l, ng * 4:(ng + 1) * 4].unsqueeze(2).broadcast_to([128, 4, 4]),
                                                      op=ALU.add), reads=[pm, bT], writes=[modT])
            for gi, c0 in enumerate((16, 40)):
                for half in range(2):
                    pb = self.P[2 + (gi * 2 + half) % 2]
                    for j in range(4):
                        c = c0 + half * 4 + j
                        k.op("pe", lambda e: e.transpose(out=pb.t[0:4, j * 128:(j + 1) * 128], in_=modT.t[:, l, c, :], identity=self.identF.t[:]),
                             reads=[modT, self.identF], writes=[pb])
                    k.op("dve", lambda e: e.tensor_copy(out=grow.t[:, (gi * 2 + half) * 512:(gi * 2 + half + 1) * 512], in_=pb.t[0:4, :]),
                         reads=[pb], writes=[grow])
            k.dma("sp", S["GROW"][l].rearrange("g v n -> v g n"), grow.t[:].rearrange("v (g n) -> v g n", g=2), grow,
                  reads=[grow], writes=[k.dk(("GROW", l))])
            for c0 in (8, 32):
                k.op("dve", lambda e: e.tensor_scalar(out=modT.t[:, l, c0:c0 + 8, :], in0=modT.t[:, l, c0:c0 + 8, :], scalar1=1.0, scalar2=None, op0=ALU.add),
                     reads=[], writes=[modT])

    def phase_proj(self, l):
        cfg, k, I, S = self.cfg, self.k, self.I, self.S
        NB = cfg.NB
        if True:
            self.win = k.sb("win", [128, 8, 3600], BF16)
            self.xt = [k.sb(f"xt{i}", [128, 1024], F32) for i in range(4)]
            self.hT = [k.sb(f"hT{i}", [128, 8, 512], BF16) for i in range(2)]
            self.ev = [k.sb(f"ev{i}", [128, 512], BF16) for i in range(4)]
            self.vaev = [k.sb(f"vaev{i}", [128, 8, 65], BF16) for i in range(2)]
            for b_ in self.vaev:
                k.op("pool", lambda e: e.memset(b_.t[:], 1.0), writes=[b_])
            self.zev = [k.sb(f"zev{i}", [128, 512], BF16) for i in range(2)]
            self.baev = [k.sb(f"baev{i}", [128, 16], F32) for i in range(2)]
            self.cnt = {"x": 0, "h": 0, "ev": 0, "tm": 0, "pb": 0}
        win = self.win
        for (c0, c1) in ((0, 1800), (1800, 3600)):
            k.dma("pool", win.t[:, :, c0:c1], I["w_in"][l, :, c0:c1].rearrange("(kc p) n -> p kc n", p=128), win, writes=[win])
        cnt = self.cnt
        for b in range(NB):
            for (t0, n, isctx) in cfg.groups():
                v = NB if isctx else b
                nt = n // 128
                hT = self.hT[cnt["h"] % 2]
                cnt["h"] += 1
                src = I["x0"] if l == 0 else S["X"]
                xts = []
                for i in range(nt):
                    xt = self.xt[cnt["x"] % 4]
                    cnt["x"] += 1
                    k.dma("sp", xt.t[:], src[b, t0 + i * 128:t0 + (i + 1) * 128, :], xt, reads=[k.dk(("X", b, (t0 // 128) + i))], writes=[xt])
                    xts.append(xt)
                for kc in range(8):
                    pb = self.P[cnt["pb"] % 8]
                    cnt["pb"] += 1
                    for i in range(nt):
                        k.op("pe", lambda e: e.transpose(out=pb.t[:, i * 128:(i + 1) * 128], in_=xts[i].t[:, kc * 128:(kc + 1) * 128], identity=self.identF.t[:]),
                             reads=[xts[i], self.identF], writes=[pb])
                    k.op("act", lambda e: e.activation(out=hT.t[:, kc, 0:n], in_=pb.t[:, 0:n], func=AF.Identity,
                                                       scale=self.modT.t[:, l, 8 + kc, v:v + 1], bias=self.modT.t[:, l, kc, v:v + 1]),
                         reads=[pb, self.modT], writes=[hT])
                for j in range(20):
                    col0 = j * 128 if j < 8 else 1536 + (j - 8) * 128
                    pb = self.P[cnt["pb"] % 8]
                    cnt["pb"] += 1
                    for kc in range(8):
                        k.op("pe", lambda e: e.matmul(pb.t[:, 0:n], win.t[:, kc, col0:col0 + 128], hT.t[:, kc, 0:n], start=(kc == 0), stop=(kc == 7)),
                             reads=[win, hT], writes=[pb])
                    ev = self.ev[cnt["ev"] % 4]
                    cnt["ev"] += 1
                    if j < 4:
                        k.op("act", lambda e: e.activation(out=ev.t[:, 0:n], in_=pb.t[:, 0:n], func=AF.Copy, scale=0.125), reads=[pb], writes=[ev])
                    elif j % 2 == 0:
                        k.op("act", lambda e: e.activation(out=ev.t[:, 0:n], in_=pb.t[:, 0:n], func=AF.Copy), reads=[pb], writes=[ev])
                    else:
                        k.op("dve", lambda e: e.tensor_copy(out=ev.t[:, 0:n], in_=pb.t[:, 0:n]), reads=[pb], writes=[ev])
                    k.dma("sp", S["FT"][b, j * 128:(j + 1) * 128, t0:t0 + n], ev.t[:, 0:n], ev, reads=[ev], writes=[k.dk(("FT", b, t0))])
                for i in range(nt):
                    ti = cnt["tm"] % 2
                    cnt["tm"] += 1
                    tok = slice(i * 128, (i + 1) * 128)
                    g0 = t0 + i * 128
                    pv, pz, pba = self.P[cnt["pb"] % 8], self.P[(cnt["pb"] + 1) % 8], self.P[(cnt["pb"] + 2) % 8]
                    cnt["pb"] += 3
                    for (pb, c0, w) in ((pv, 1024, 512), (pz, 3072, 512), (pba, 3584, 16)):
                        for kc in range(8):
                            k.op("pe", lambda e: e.matmul(pb.t[:, 0:w], hT.t[:, kc, tok], win.t[:, kc, c0:c0 + w], start=(kc == 0), stop=(kc == 7)),
                                 reads=[win, hT], writes=[pb])
                    va, ze, ba = self.vaev[ti], self.zev[ti], self.baev[ti]
                    k.op("dve", lambda e: e.tensor_copy(out=va.t[:, :, 0:64], in_=pv.t[:, :].rearrange("p (h d) -> p h d", d=64)), reads=[pv], writes=[va])
                    k.dma("sp", S["VA"][b, g0:g0 + 128], va.t[:], va, reads=[va], writes=[k.dk(("VA", b, g0))])
                    k.op("act", lambda e: e.activation(out=ze.t[:], in_=pz.t[:], func=AF.Silu), reads=[pz], writes=[ze])
                    k.dma("sp", S["ZS"][b, g0:g0 + 128, :], ze.t[:], ze, reads=[ze], writes=[k.dk(("ZS", b, g0))])
                    k.op("dve", lambda e: e.tensor_copy(out=ba.t[:], in_=pba.t[:, 0:16]), reads=[pba], writes=[ba])
                    k.dma("sp", S["BA"][b, g0:g0 + 128, :], ba.t[:], ba, reads=[ba], writes=[k.dk(("BA", b, g0))])


def na_plan(cfg):
    ROWS = cfg.ROWS
    rs = lambda r: min(max(r - 4, 0), ROWS - 8)
    planes = {}
    blocks = []
    for R in range(ROWS // 8):
        rows = list(range(8 * R, 8 * R + 8))
        lo, hi = rs(rows[0]), rs(rows[-1]) + 7
        krs = []
        for kr in range(lo, hi + 1):
            sig = tuple((kr - r + 7) if rs(r) <= kr <= rs(r) + 7 else None for r in rows)
            if sig not in planes:
                planes[sig] = len(planes)
            krs.append((kr, planes[sig], sig))
        blocks.append((lo, hi, krs))
    return blocks, planes


def na_tables(cfg, rpb):
    blocks, planes = na_plan(cfg)
    L = rpb.shape[0]
    NPL = len(planes)
    kc = np.arange(64)[:, None]
    qc = np.arange(64)[None, :]
    cstart = np.clip(qc - 8, 0, 48)
    col_ok = (kc >= cstart) & (kc < cstart + 16)
    dc = np.clip(kc - qc + 15, 0, 30)
    BT = np.full((L, 8, 64, NPL, 8, 64), -30000.0, np.float32)
    for sig, pi in planes.items():
        for ri, dr in enumerate(sig):
            if dr is None:
                continue
            vals = rpb[:, :, dr, :][:, :, dc]
            BT[:, :, :, pi, ri, :] = np.where(col_ok[None, None], vals, np.float32(-30000.0))
    return BT


def phase_na(self, l):
    cfg, k, I, S = self.cfg, self.k, self.I, self.S
    NB, CTX, NTC = cfg.NB, cfg.CTX, cfg.NTC
    blocks, planes = na_plan(cfg)
    NPL = len(planes)
    if True:
        self.bth = k.sb("bth", [64, NPL, 512], BF16)
        self.na_qt = [k.sb(f"naq{i}", [64, 512], BF16) for i in range(2)]
        self.na_kt = [k.sb(f"nak{i}", [64, 15 * 64], BF16) for i in range(2)]
        self.na_vt = [k.sb(f"nav{i}", [64, 15, 65], BF16) for i in range(2)]
        self.na_kc = [k.sb(f"nakc{i}", [64, CTX], BF16) for i in range(2)]
        self.na_vc = [k.sb(f"navc{i}", [128, NTC, 65], BF16) for i in range(2)]
        self.na_qc = [k.sb(f"naqc{i}", [64, CTX], BF16) for i in range(2)]
        self.na_pt = [[k.sb(f"napt{s}_{i}", [64, 512], BF16) for i in range(15)] for s in range(2)]
        self.na_pc = [[k.sb(f"napc{s}_{i}", [128, 512], BF16) for i in range(NTC)] for s in range(2)]
        self.na_rec = [k.sb(f"narec{i}", [128, 4], F32) for i in range(2)]
        self.na_o = [k.sb(f"nao{i}", [128, 4, 64], F32) for i in range(2)]
        self.nac = {"s": 0, "o": 0, "hb": 0, "blk": 0}
    c = self.nac
    SP = self.P[0:4]
    OP = self.P[4:6]

    def spb():
        c["s"] += 1
        return SP[c["s"] % 4]

    def attend(nq, qt_ap, locs, pcs, vc, dst_fn):
        ob = OP[c["o"] % 2]
        rec = self.na_rec[c["o"] % 2]
        ot = self.na_o[c["o"] % 2]
        c["o"] += 1
        ng = nq // 128
        for g in range(ng):
            ops = []
            for (pt, vt, ki, valid) in locs:
                if valid[g]:
                    ops.append((pt.t[:, g * 128:(g + 1) * 128], vt.t[:, ki, :], [pt, vt]))
            for ci, pc in enumerate(pcs):
                ops.append((pc.t[:, g * 128:(g + 1) * 128], vc.t[:, ci, :], [pc, vc]))
            for oi, (lh, rh, rd) in enumerate(ops):
                k.op("pe", lambda e: e.matmul(ob.t[:, g * 65:(g + 1) * 65], lh, rh, start=(oi == 0), stop=(oi == len(ops) - 1)), reads=rd, writes=[ob])
        ov = ob.t[:, 0:ng * 65].rearrange("p (g e) -> p g e", e=65)
        k.op("dve", lambda e: e.reciprocal(out=rec.t[:, 0:ng], in_=ov[:, :, 64]), reads=[ob], writes=[rec])
        k.op("dve", lambda e: e.tensor_tensor(out=ot.t[:, 0:ng, :], in0=ov[:, :, 0:64], in1=rec.t[:, 0:ng].unsqueeze(2).broadcast_to([128, ng, 64]), op=ALU.mult),
             reads=[ob, rec], writes=[ot])
        dst, dkey = dst_fn()
        k.dma("sp", dst, ot.t[:, 0:ng, :], ot, reads=[ot], writes=[dkey])

    for h in range(8):
        k.dma("pool", self.bth.t[:], I["BT"][l, h].rearrange("p n r q -> p n (r q)"), self.bth, writes=[self.bth])
        for b in range(NB):
            hb = c["hb"] % 2
            c["hb"] += 1
            kcT, vc, qcT = self.na_kc[hb], self.na_vc[hb], self.na_qc[hb]
            ftk = k.dk(("FT", b, 0))
            k.dma("sp", kcT.t[:], S["FT"][b, 512 + 64 * h:512 + 64 * h + 64, 0:CTX], kcT, reads=[ftk], writes=[kcT])
            k.dma("sp", qcT.t[:], S["FT"][b, 64 * h:64 * h + 64, 0:CTX], qcT, reads=[ftk], writes=[qcT])
            k.dma("sp", vc.t[:], S["VA"][b, 0:CTX, h, :].rearrange("(c p) e -> p c e", p=128), vc, reads=[k.dk(("VA", b, i * 128)) for i in range(NTC)], writes=[vc])
            st = c["blk"] % 2
            c["blk"] += 1
            pcs = self.na_pc[st]
            for ci in range(NTC):
                pb = spb()
                k.op("pe", lambda e: e.matmul(pb.t[:, 0:CTX], kcT.t[:, ci * 128:(ci + 1) * 128], qcT.t[:, :], start=True, stop=True), reads=[kcT, qcT], writes=[pb])
                k.op("act", lambda e: e.activation(out=pcs[ci].t[:, 0:CTX], in_=pb.t[:, 0:CTX], func=AF.Exp), reads=[pb], writes=[pcs[ci]])
            attend(CTX, None, [], pcs, vc,
                   lambda: (S["ONA"][b, 0:CTX, 64 * h:64 * h + 64].rearrange("(g p) d -> p g d", p=128), k.dk(("ONA", b, h, -1))))
            for R, (lo, hi, krs) in enumerate(blocks):
                st = c["blk"] % 2
                c["blk"] += 1
                qt, kt, vt = self.na_qt[st], self.na_kt[st], self.na_vt[st]
                nkr = hi - lo + 1
                q0 = CTX + R * 512
                k0 = CTX + lo * 64
                ft_keys = [k.dk(("FT", b, t0)) for (t0, n, isc) in cfg.groups()]
                va_keys = [k.dk(("VA", b, (k0 // 128) * 128 + i * 128)) for i in range((nkr * 64 + 127) // 128 + 1)]
                k.dma("sp", qt.t[:], S["FT"][b, 64 * h:64 * h + 64, q0:q0 + 512], qt, reads=ft_keys, writes=[qt])
                k.dma("sp", kt.t[:, 0:nkr * 64], S["FT"][b, 512 + 64 * h:512 + 64 * h + 64, k0:k0 + nkr * 64], kt, reads=ft_keys, writes=[kt])
                k.dma("sp", vt.t[:, 0:nkr, :], S["VA"][b, k0:k0 + nkr * 64, h, :].rearrange("(r p) e -> p r e", p=64), vt, reads=va_keys, writes=[vt])
                pts = self.na_pt[st]
                pcs = self.na_pc[st]
                locs = []
                for ki, (kr, pi, sig) in enumerate(krs):
                    pb = spb()
                    k.op("pe", lambda e: e.matmul(pb.t[0:64, :], kt.t[:, ki * 64:(ki + 1) * 64], qt.t[:, :], start=True, stop=False), reads=[kt, qt], writes=[pb])
                    k.op("pe", lambda e: e.matmul(pb.t[0:64, :], self.identB.t[0:64, 0:64], self.bth.t[:, pi, :], start=False, stop=True), reads=[self.identB, self.bth], writes=[pb])
                    k.op("act", lambda e: e.activation(out=pts[ki].t[:, :], in_=pb.t[0:64, :], func=AF.Exp), reads=[pb], writes=[pts[ki]])
                    valid = [(sig[2 * g] is not None) or (sig[2 * g + 1] is not None) for g in range(4)]
                    locs.append((pts[ki], vt, ki, valid))
                for ci in range(NTC):
                    pb = spb()
                    k.op("pe", lambda e: e.matmul(pb.t[:, :], kcT.t[:, ci * 128:(ci + 1) * 128], qt.t[:, :], start=True, stop=True), reads=[kcT, qt], writes=[pb])
                    k.op("act", lambda e: e.activation(out=pcs[ci].t[:, :], in_=pb.t[:, :], func=AF.Exp), reads=[pb], writes=[pcs[ci]])
                attend(512, None, locs, pcs, vc,
                       lambda: (S["ONA"][b, q0:q0 + 512, 64 * h:64 * h + 64].rearrange("(g p) d -> p g d", p=128), k.dk(("ONA", b, h, R))))


Prog.phase_na = phase_na


def gdn_consts(cfg):
    idx = np.arange(128)
    same = (idx[:, None] // 64) == (idx[None, :] // 64)
    c = {}
    Ef = same & (idx[:, None] <= idx[None, :])
    Eb = same & (idx[:, None] >= idx[None, :])
    Ff = same & (idx[:, None] > idx[None, :])
    Fb = same & (idx[:, None] < idx[None, :])
    sel0 = np.broadcast_to((idx[:, None] // 64) == 0, (128, 128))
    sel1 = np.broadcast_to((idx[:, None] // 64) == 1, (128, 128))
    gm = np.stack([Ef, Eb, Ff, Fb, sel0, sel1, np.ones((128, 128), bool), np.zeros((128, 128), bool)], 1).astype(np.float32)
    c["gmask"] = np.ascontiguousarray(gm)
    mb = np.stack([(Ef.astype(np.float32) - 1.0) * 30000.0, (Eb.astype(np.float32) - 1.0) * 30000.0,
                   -(Ef & ~np.eye(128, dtype=bool)).astype(np.float32), -(Eb & ~np.eye(128, dtype=bool)).astype(np.float32)], 1)
    c["gmb"] = np.ascontiguousarray(mb.astype(np.float32))
    nf = 32
    inv = (10000.0 ** (-np.arange(nf, dtype=np.float32) / nf)).astype(np.float32)
    t = np.arange(cfg.SEQ)
    rows = (t // 64).astype(np.float32)
    cols = (t % 64).astype(np.float32)
    ang = np.concatenate([rows[:, None] * inv[None, :], cols[:, None] * inv[None, :]], 1).astype(np.float32)
    NTL = cfg.SEQ // 128
    c["ropec"] = np.ascontiguousarray(np.cos(ang).astype(np.float32).reshape(NTL, 128, 64).transpose(1, 0, 2))
    c["ropes"] = np.ascontiguousarray(np.sin(ang).astype(np.float32).reshape(NTL, 128, 64).transpose(1, 0, 2))
    return c


def phase_gdn_a(self, l):
    cfg, k, I, S = self.cfg, self.k, self.I, self.S
    NB, CTX, NTC, NT = cfg.NB, cfg.CTX, cfg.NTC, cfg.NT
    NTL = NT - NTC
    P = self.P
    pc = {"i": 0}

    def nextP():
        pc["i"] += 1
        return P[pc["i"] % 7]

    gm = k.sb("gm", [128, 8, 128], F32)
    k.dma("sp", gm.t[:], I["gmask"][:, :, :], gm, writes=[gm])
    gmb = k.sb("gmb", [128, 4, 128], F32)
    k.dma("sp", gmb.t[:], I["gmb"][:, :, :], gmb, writes=[gmb])
    ropec = k.sb("ropec", [128, NTL, 64], F32)
    ropes = k.sb("ropes", [128, NTL, 64], F32)
    k.dma("sp", ropec.t[:], I["ropec"][:, :, :], ropec, writes=[ropec])
    k.dma("sp", ropes.t[:], I["ropes"][:, :, :], ropes, writes=[ropes])
    cw = k.sb("cw", [128, 12, 5], F32)
    k.dma("sp", cw.t[:], I["cwT"][:, l], cw, writes=[cw])
    dw = k.sb("dw", [128, 12, 5, 128], BF16)
    for cc in range(12):
        for s in range(5):
            k.op("dve", lambda e: e.tensor_scalar(out=dw.t[:, cc, s, :], in0=self.identF.t[:], scalar1=cw.t[:, cc, s:s + 1], scalar2=None, op0=ALU.mult),
                 reads=[cw, self.identF], writes=[dw])
    alog = k.sb("alog", [128, 8], F32)
    dtb = k.sb("dtb", [128, 8], F32)
    k.dma("sp", alog.t[:], I["a_log"][l:l + 1, :].partition_broadcast(128) if False else I["a_log"][l:l + 1, :].broadcast_to([128, 8]), alog, writes=[alog])
    k.dma("sp", dtb.t[:], I["dt_bias"][l:l + 1, :].broadcast_to([128, 8]), dtb, writes=[dtb])
    nea = k.sb("nea", [128, 8], F32)
    k.op("act", lambda e: e.activation(out=nea.t[:], in_=alog.t[:], func=AF.Exp), reads=[alog], writes=[nea])
    k.op("dve", lambda e: e.tensor_scalar(out=nea.t[:], in0=nea.t[:], scalar1=-1.0, scalar2=None, op0=ALU.mult), reads=[nea], writes=[nea])

    xc = [k.sb(f"xc{i}", [128, 12, 516], BF16) for i in range(2)]
    sil = [k.sb(f"sil{i}", [128, 512], BF16) for i in range(2)]
    tma = [k.sb(f"tma{i}", [128, 4, 12, 128], F32) for i in range(1)]
    NS = 2

    def mk(name, shape, dt, n=NS):
        return [k.sb(f"{name}{i}", shape, dt) for i in range(n)]

    sqt = mk("sqt", [128, 8, 128], F32, 1)
    ss = mk("ss", [128, 8], F32)
    qkn = mk("qkn", [128, 8, 128], F32)
    qkr = mk("qkr", [128, 8, 128], F32)
    rt1 = mk("rt1", [128, 8, 2, 32], F32, 1)
    rt2 = mk("rt2", [128, 8, 2, 32], F32, 1)
    qkb = mk("qkb", [128, 8, 128], BF16)
    bat = mk("bat", [128, 16], F32)
    gsm = mk("gsm", [128, 6, 8], F32)
    ex = mk("ex", [128, 32], F32)
    tok16 = mk("tok16", [128, 5, 8, 128], BF16)
    ft16 = mk("ft16", [128, 24, 128], BF16)
    rhs1 = mk("rhs1", [128, 8, 128], F32)
    rhs2 = mk("rhs2", [128, 8, 128], F32)
    dtmp = mk("dtmp", [128, 4, 128], F32)
    dec = mk("dec", [128, 4, 128], F32)
    decn = mk("decn", [128, 4, 128], F32)
    mt = mk("mt", [128, 4, 128], BF16, 3)
    mm = mk("mm", [128, 4, 128], BF16, 3)
    rt = mk("rt", [128, 4, 128], BF16, 3)
    g16 = mk("g16", [128, 4, 4, 128], BF16, 2)
    g32 = mk("g32", [128, 520], F32, 2)
    cn = {"g": 0, "t": 0, "u": 0, "m": 0}

    for b in range(NB):
        for (t0, n, isctx) in cfg.groups():
            s0, s1 = (0, CTX) if isctx else (CTX, cfg.TOK)
            nt = n // 128
            gi = cn["g"] % 2
            cn["g"] += 1
            xcb = xc[gi]
            lo, hi = max(t0 - 2, s0), min(t0 + n + 2, s1)
            if lo != t0 - 2 or hi != t0 + n + 2:
                k.op("pool", lambda e: e.memset(xcb.t[:], 0.0), writes=[xcb])
            k.dma("sp", xcb.t[:, :, lo - (t0 - 2):hi - (t0 - 2)], S["FT"][b, 1024:2560, lo:hi].rearrange("(c p) t -> p c t", p=128), xcb,
                  reads=[k.dk(("FT", b, g_[0])) for g_ in cfg.groups()], writes=[xcb])
            tm = tma[0]
            for cc in range(12):
                pb = nextP()
                for s in range(5):
                    k.op("pe", lambda e: e.matmul(pb.t[:, 0:n], dw.t[:, cc, s, :], xcb.t[:, cc, s:s + n], start=(s == 0), stop=(s == 4)), reads=[dw, xcb], writes=[pb])
                sl = sil[cc % 2]
                k.op("act", lambda e: e.activation(out=sl.t[:, 0:n], in_=pb.t[:, 0:n], func=AF.Silu), reads=[pb], writes=[sl])
                pt = nextP()
                ptb = pt.t[:].bitcast(BF16)
                for i in range(nt):
                    k.op("pe", lambda e: e.transpose(out=ptb[:, i * 128:(i + 1) * 128], in_=sl.t[:, i * 128:(i + 1) * 128], identity=self.identB.t[:]),
                         reads=[sl, self.identB], writes=[pt])
                k.op("dve", lambda e: e.tensor_copy(out=tm.t[:, 0:nt, cc, :], in_=ptb[:, 0:nt * 128].rearrange("p (i d) -> p i d", d=128)), reads=[pt], writes=[tm])
            for i in range(nt):
                ti = t0 // 128 + i
                u = cn["t"] % NS
                cn["t"] += 1
                T = tm.t[:, i]
                k.op("dve", lambda e: e.tensor_tensor(out=sqt[0].t[:], in0=T[:, 0:8, :], in1=T[:, 0:8, :], op=ALU.mult), reads=[tm], writes=[sqt[0]])
                k.op("dve", lambda e: e.tensor_reduce(out=ss[u].t[:], in_=sqt[0].t[:], axis=AX.X, op=ALU.add), reads=[sqt[0]], writes=[ss[u]])
                k.op("dve", lambda e: e.tensor_scalar(out=ss[u].t[:], in0=ss[u].t[:], scalar1=1e-6, scalar2=None, op0=ALU.add), reads=[ss[u]], writes=[ss[u]])
                k.op("act", lambda e: e.activation(out=ss[u].t[:], in_=ss[u].t[:], func=AF.Sqrt), reads=[ss[u]], writes=[ss[u]])
                k.op("dve", lambda e: e.reciprocal(out=ss[u].t[:], in_=ss[u].t[:]), reads=[ss[u]], writes=[ss[u]])
                k.op("dve", lambda e: e.tensor_scalar(out=ss[u].t[:, 0:4], in0=ss[u].t[:, 0:4], scalar1=float(128 ** -0.5), scalar2=None, op0=ALU.mult), reads=[ss[u]], writes=[ss[u]])
                k.op("dve", lambda e: e.tensor_tensor(out=qkn[u].t[:], in0=T[:, 0:8, :], in1=ss[u].t[:].unsqueeze(2).broadcast_to([128, 8, 128]), op=ALU.mult),
                     reads=[tm, ss[u]], writes=[qkn[u]])
                if isctx:
                    qk_ = qkn[u]
                else:
                    tl = ti - NTC
                    x5 = qkn[u].t[:].rearrange("p h (a c f) -> p h a c f", a=2, c=2)
                    o5 = qkr[u].t[:].rearrange("p h (a c f) -> p h a c f", a=2, c=2)
                    cosb = ropec.t[:, tl, :].rearrange("p (a f) -> p a f", a=2).unsqueeze(1).broadcast_to([128, 8, 2, 32])
                    sinb = ropes.t[:, tl, :].rearrange("p (a f) -> p a f", a=2).unsqueeze(1).broadcast_to([128, 8, 2, 32])
                    xA, xB = x5[:, :, :, 0, :], x5[:, :, :, 1, :]
                    k.op("dve", lambda e: e.tensor_tensor(out=rt1[0].t[:], in0=xA, in1=cosb, op=ALU.mult), reads=[qkn[u], ropec], writes=[rt1[0]])
                    k.op("dve", lambda e: e.tensor_tensor(out=rt2[0].t[:], in0=xB, in1=sinb, op=ALU.mult), reads=[qkn[u], ropes], writes=[rt2[0]])
                    k.op("dve", lambda e: e.tensor_tensor(out=o5[:, :, :, 0, :], in0=rt1[0].t[:], in1=rt2[0].t[:], op=ALU.subtract), reads=[rt1[0], rt2[0]], writes=[qkr[u]])
                    k.op("dve", lambda e: e.tensor_tensor(out=rt1[0].t[:], in0=xA, in1=sinb, op=ALU.mult), reads=[qkn[u], ropes], writes=[rt1[0]])
                    k.op("dve", lambda e: e.tensor_tensor(out=rt2[0].t[:], in0=xB, in1=cosb, op=ALU.mult), reads=[qkn[u], ropec], writes=[rt2[0]])
                    k.op("dve", lambda e: e.tensor_tensor(out=o5[:, :, :, 1, :], in0=rt1[0].t[:], in1=rt2[0].t[:], op=ALU.add), reads=[rt1[0], rt2[0]], writes=[qkr[u]])
                    qk_ = qkr[u]
                k.op("act", lambda e: e.activation(out=qkb[u].t[:], in_=qk_.t[:], func=AF.Copy), reads=[qk_], writes=[qkb[u]])
                k.dma("sp", bat[u].t[:], S["BA"][b, ti * 128:(ti + 1) * 128, :], bat[u], reads=[k.dk(("BA", b, ti * 128))], writes=[bat[u]])
                G = gsm[u]
                beta, xx, nx, lp, g, bsc = [G.t[:, j, :] for j in range(6)]
                k.op("act", lambda e: e.activation(out=beta, in_=bat[u].t[:, 0:8], func=AF.Sigmoid), reads=[bat[u]], writes=[G])
                k.op("dve", lambda e: e.tensor_tensor(out=xx, in0=bat[u].t[:, 8:16], in1=dtb.t[:], op=ALU.add), reads=[bat[u], dtb], writes=[G])
                k.op("dve", lambda e: e.tensor_scalar(out=lp, in0=xx, scalar1=0.0, scalar2=None, op0=ALU.max), reads=[G], writes=[G])
                k.op("dve", lambda e: e.scalar_tensor_tensor(out=nx, in0=lp, scalar=-2.0, in1=xx, op0=ALU.mult, op1=ALU.add), reads=[G], writes=[G])
                k.op("act", lambda e: e.activation(out=nx, in_=nx, func=AF.Exp), reads=[G], writes=[G])
                k.op("act", lambda e: e.activation(out=nx, in_=nx, func=AF.Ln, bias=1.0), reads=[G], writes=[G])
                k.op("dve", lambda e: e.tensor_tensor(out=lp, in0=lp, in1=nx, op=ALU.add), reads=[G], writes=[G])
                k.op("dve", lambda e: e.tensor_tensor(out=g, in0=lp, in1=nea.t[:], op=ALU.mult), reads=[G, nea], writes=[G])
                pg = nextP()
                for d in range(2):
                    k.op("pe", lambda e: e.matmul(pg.t[:, d * 4:(d + 1) * 4], gm.t[:, d, :], G.t[:, 4, d * 4:(d + 1) * 4], start=True, stop=True), reads=[gm, G], writes=[pg])
                    k.op("pe", lambda e: e.matmul(pg.t[:, 8 + d * 4:8 + (d + 1) * 4], gm.t[:, 2 + d, :], G.t[:, 4, d * 4:(d + 1) * 4], start=True, stop=True), reads=[gm, G], writes=[pg])
                for c in range(2):
                    k.op("pe", lambda e: e.matmul(pg.t[:, 16 + c * 8:16 + (c + 1) * 8], gm.t[:, 4 + c, :], G.t[:, 4, :], start=True, stop=True), reads=[gm, G], writes=[pg])
                EX = ex[u]
                k.op("act", lambda e: e.activation(out=EX.t[:], in_=pg.t[:, 0:32], func=AF.Exp), reads=[pg], writes=[EX])
                k.op("dve", lambda e: e.tensor_tensor(out=bsc, in0=beta, in1=EX.t[:, 0:8], op=ALU.mult), reads=[G, EX], writes=[G])
                T16 = tok16[u]
                Kh = qk_.t[:, 4:8, :].unsqueeze(1).broadcast_to([128, 2, 4, 128])
                Qh = qk_.t[:, 0:4, :].unsqueeze(1).broadcast_to([128, 2, 4, 128])
                Vh = T[:, 8:12, :].unsqueeze(1).broadcast_to([128, 2, 4, 128])

                def sc8(ap):
                    return ap.rearrange("p (d h) -> p d h", d=2).unsqueeze(3).broadcast_to([128, 2, 4, 128])

                for j, (src, scl, rd) in enumerate(((Kh, beta, [G]), (Kh, bsc, [G]), (Kh, EX.t[:, 8:16], [EX]), (Vh, beta, [G, tm]), (Qh, EX.t[:, 0:8], [EX]))):
                    eng = "dve" if j % 2 == 0 else "pool"
                    k.op(eng, lambda e: e.tensor_tensor(out=T16.t[:, j].rearrange("p (d h) x -> p d h x", d=2), in0=src, in1=sc8(scl), op=ALU.mult),
                         reads=[qk_] + rd, writes=[T16])
                F16 = ft16[u]
                srcs = [qkb[u].t[:, 4 + h, :] for h in range(4)] + [qkb[u].t[:, h, :] for h in range(4)] + [T16.t[:, 0, j, :] for j in range(8)] + [T16.t[:, 4, j, :] for j in range(8)]
                for q8 in range(3):
                    pt = nextP()
                    ptb = pt.t[:].bitcast(BF16)
                    for j in range(8):
                        k.op("pe", lambda e: e.transpose(out=ptb[:, j * 128:(j + 1) * 128], in_=srcs[q8 * 8 + j], identity=self.identB.t[:]),
                             reads=[qkb[u], T16, self.identB], writes=[pt])
                    eng = "act" if q8 % 2 == 0 else "dve"
                    if eng == "act":
                        k.op("act", lambda e: e.activation(out=F16.t[:, q8 * 8:(q8 + 1) * 8, :], in_=ptb.rearrange("p (j x) -> p j x", x=128), func=AF.Copy), reads=[pt], writes=[F16])
                    else:
                        k.op("dve", lambda e: e.tensor_copy(out=F16.t[:, q8 * 8:(q8 + 1) * 8, :], in_=ptb.rearrange("p (j x) -> p j x", x=128)), reads=[pt], writes=[F16])
                R1, R2 = rhs1[u], rhs2[u]
                for d in range(2):
                    k.op("pool", lambda e: e.tensor_tensor(out=R1.t[:, d * 4:(d + 1) * 4, :], in0=gm.t[:, d, :].unsqueeze(1).broadcast_to([128, 4, 128]),
                                                           in1=G.t[:, 4, d * 4:(d + 1) * 4].unsqueeze(2).broadcast_to([128, 4, 128]), op=ALU.mult), reads=[gm, G], writes=[R1])
                k.op("act", lambda e: e.activation(out=R2.t[:], in_=G.t[:, 4, :].unsqueeze(2).broadcast_to([128, 8, 128]), func=AF.Copy, scale=-1.0), reads=[G], writes=[R2])
                pqk = P[7]
                for h in range(4):
                    k.op("pe", lambda e: e.matmul(pqk.t[:, h * 128:(h + 1) * 128], F16.t[:, h, :], F16.t[:, 4 + h, :], start=True, stop=True), reads=[F16], writes=[pqk])
                for d in range(2):
                    mu = d
                    gu = cn["u"] % 2
                    cn["u"] += 1
                    pgb = nextP()
                    for h in range(4):
                        k.op("pe", lambda e: e.matmul(pgb.t[:, h * 128:(h + 1) * 128], F16.t[:, h, :], F16.t[:, 8 + d * 4 + h, :], start=True, stop=True), reads=[F16], writes=[pgb])
                    pd = nextP()
                    k.op("pe", lambda e: e.matmul(pd.t[:, :], gm.t[:, 6, :], R1.t[:, d * 4:(d + 1) * 4, :], start=True, stop=False), reads=[gm, R1], writes=[pd])
                    k.op("pe", lambda e: e.matmul(pd.t[:, :], gm.t[:, d, :], R2.t[:, d * 4:(d + 1) * 4, :], start=False, stop=True), reads=[gm, R2], writes=[pd])
                    DT, DE, DN = dtmp[mu], dec[mu], decn[mu]
                    k.op("dve", lambda e: e.scalar_tensor_tensor(out=DT.t[:], in0=pd.t[:, :].rearrange("p (h x) -> p h x", x=128), scalar=0.0,
                                                                 in1=gmb.t[:, d, :].unsqueeze(1).broadcast_to([128, 4, 128]), op0=ALU.min, op1=ALU.add), reads=[pd, gmb], writes=[DT])
                    k.op("act", lambda e: e.activation(out=DE.t[:], in_=DT.t[:], func=AF.Exp), reads=[DT], writes=[DE])
                    k.op("pool", lambda e: e.tensor_tensor(out=DN.t[:], in0=DE.t[:], in1=gmb.t[:, 2 + d, :].unsqueeze(1).broadcast_to([128, 4, 128]), op=ALU.mult), reads=[DE, gmb], writes=[DN])
                    G16, G32 = g16[gu], g32[gu]
                    def nm():
                        cn["m"] += 1
                        return cn["m"] % 3
                    MT = mt[nm()]
                    k.op("dve", lambda e: e.tensor_tensor(out=MT.t[:], in0=pgb.t[:, :].rearrange("p (h x) -> p h x", x=128), in1=DN.t[:], op=ALU.mult), reads=[pgb, DN], writes=[MT])
                    k.op("dve", lambda e: e.tensor_tensor(out=G16.t[:, 1], in0=pqk.t[:, :].rearrange("p (h x) -> p h x", x=128), in1=DE.t[:], op=ALU.mult), reads=[pqk, DE], writes=[G16])
                    pt = nextP()
                    ptb = pt.t[:].bitcast(BF16)
                    for h in range(4):
                        k.op("pe", lambda e: e.transpose(out=ptb[:, h * 128:(h + 1) * 128], in_=MT.t[:, h, :], identity=self.identB.t[:]), reads=[MT, self.identB], writes=[pt])
                    M = mm[cn["m"] % 3]
                    k.op("act", lambda e: e.activation(out=M.t[:], in_=ptb[:, 0:512].rearrange("p (h x) -> p h x", x=128), func=AF.Copy), reads=[pt], writes=[M])
                    RT = rt[cn["m"] % 3]
                    k.op("pool", lambda e: e.tensor_tensor(out=RT.t[:], in0=MT.t[:], in1=self.identB.t[:].unsqueeze(1).broadcast_to([128, 4, 128]), op=ALU.add), reads=[MT, self.identB], writes=[RT])
                    for step in range(5):
                        p1 = nextP()
                        for h in range(4):
                            k.op("pe", lambda e: e.matmul(p1.t[:, h * 128:(h + 1) * 128], MT.t[:, h, :], M.t[:, h, :], start=True, stop=True), reads=[MT, M], writes=[p1])
                        if step < 4:
                            p2 = nextP()
                            for h in range(4):
                                k.op("pe", lambda e: e.matmul(p2.t[:, h * 128:(h + 1) * 128], M.t[:, h, :], MT.t[:, h, :], start=True, stop=True), reads=[MT, M], writes=[p2])
                        ni = nm()
                        M2, MT2, RT2 = mm[ni], mt[ni], rt[ni]
                        k.op("act", lambda e: e.activation(out=M2.t[:], in_=p1.t[:, :].rearrange("p (h x) -> p h x", x=128), func=AF.Copy), reads=[p1], writes=[M2])
                        if step < 4:
                            k.op("dve", lambda e: e.tensor_copy(out=MT2.t[:], in_=p2.t[:, :].rearrange("p (h x) -> p h x", x=128)), reads=[p2], writes=[MT2])
                        p3 = nextP()
                        for h in range(4):
                            k.op("pe", lambda e: e.matmul(p3.t[:, h * 128:(h + 1) * 128], M2.t[:, h, :], RT.t[:, h, :], start=True, stop=True), reads=[M2, RT], writes=[p3])
                        k.op("dve", lambda e: e.tensor_tensor(out=RT2.t[:], in0=p3.t[:, :].rearrange("p (h x) -> p h x", x=128), in1=RT.t[:], op=ALU.add), reads=[p3, RT], writes=[RT2])
                        M, MT, RT = M2, MT2, RT2
                    pw = nextP()
                    for h in range(4):
                        k.op("pe", lambda e: e.matmul(pw.t[:, h * 128:(h + 1) * 128], T16.t[:, 1, d * 4 + h, :], RT.t[:, h, :], start=True, stop=True), reads=[T16, RT], writes=[pw])
                    k.op("act", lambda e: e.activation(out=G16.t[:, 0], in_=pw.t[:, :].rearrange("p (h x) -> p h x", x=128), func=AF.Copy, scale=-1.0), reads=[pw], writes=[G16])
                    pu = nextP()
                    for h in range(4):
                        k.op("pe", lambda e: e.matmul(pu.t[:, h * 128:(h + 1) * 128], RT.t[:, h, :], T16.t[:, 3, d * 4 + h, :], start=True, stop=True), reads=[T16, RT], writes=[pu])
                    k.op("dve", lambda e: e.tensor_copy(out=G32.t[:, 0:512], in_=pu.t[:, :]), reads=[pu], writes=[G32])
                    k.op("pool", lambda e: e.tensor_copy(out=G16.t[:, 2], in_=F16.t[:, 16 + d * 4:16 + d * 4 + 4, :]), reads=[F16], writes=[G16])
                    k.op("pool", lambda e: e.tensor_copy(out=G16.t[:, 3], in_=T16.t[:, 2, d * 4:d * 4 + 4, :]), reads=[T16], writes=[G16])
                    k.op("dve", lambda e: e.tensor_copy(out=G32.t[:, 512:520].rearrange("p (c h) -> p c h", c=2), in_=EX.t[:, 16:32].rearrange("p (c j) -> p c j", c=2)[:, :, d * 4:(d + 1) * 4]),
                         reads=[EX], writes=[G32])
                    k.dma("sp", S["G16"][b, ti, d], G16.t[:].rearrange("p a h x -> p (a h x)"), G16, reads=[G16], writes=[k.dk(("G", b, ti, d))])
                    k.dma("sp", S["G32"][b, ti, d], G32.t[:], G32, reads=[G32], writes=[k.dk(("G", b, ti, d))])


Prog.phase_gdn_a = phase_gdn_a


def phase_gdn_b(self, l):
    cfg, k, I, S = self.cfg, self.k, self.I, self.S
    NB, CTX, NTC, NT = cfg.NB, cfg.CTX, cfg.NTC, cfg.NT
    P = self.P
    pc = {"i": 0}

    def nextP():
        pc["i"] += 1
        return P[pc["i"] % 8]

    chains = []
    for b in range(NB):
        for d in range(2):
            if d == 0:
                order = list(range(NT))
            else:
                order = list(range(NTC - 1, -1, -1)) + list(range(NT - 1, NTC - 1, -1))
            ci = len(chains)
            ch = dict(b=b, d=d, order=order,
                      S=k.sb(f"gS{ci}", [128, 4, 128], F32), Sb=k.sb(f"gSb{ci}", [128, 4, 128], BF16),
                      g16=[k.sb(f"gg16_{ci}_{i}", [128, 4, 4, 128], BF16) for i in range(2)],
                      g32=[k.sb(f"gg32_{ci}_{i}", [128, 520], F32) for i in range(2)],
                      vn=k.sb(f"gvn{ci}", [128, 4, 128], BF16),
                      o=[k.sb(f"go{ci}_{i}", [128, 512], F32) for i in range(2)])
            k.op("pool", lambda e: e.memset(ch["S"].t[:], 0.0), writes=[ch["S"]])
            k.op("pool", lambda e: e.memset(ch["Sb"].t[:], 0.0), writes=[ch["Sb"]])
            chains.append(ch)
    for step in range(NT):
        for ch in chains:
            b, d = ch["b"], ch["d"]
            ti = ch["order"][step]
            G16, G32, O = ch["g16"][step % 2], ch["g32"][step % 2], ch["o"][step % 2]
            Sf, Sb, vn = ch["S"], ch["Sb"], ch["vn"]
            k.dma("sp", G16.t[:].rearrange("p a h x -> p (a h x)"), S["G16"][b, ti, d], G16, reads=[k.dk(("G", b, ti, d))], writes=[G16])
            k.dma("sp", G32.t[:], S["G32"][b, ti, d], G32, reads=[k.dk(("G", b, ti, d))], writes=[G32])
            for c in ((0, 1) if d == 0 else (1, 0)):
                cs = slice(c * 64, (c + 1) * 64)
                pv = nextP()
                for h in range(4):
                    k.op("pe", lambda e: e.matmul(pv.t[cs, h * 128:(h + 1) * 128], G16.t[:, 0, h, cs], Sb.t[:, h, :], start=True, stop=True), reads=[G16, Sb], writes=[pv])
                k.op("dve", lambda e: e.tensor_tensor(out=vn.t[cs].rearrange("p h x -> p (h x)"), in0=pv.t[cs, :], in1=G32.t[cs, 0:512], op=ALU.add), reads=[pv, G32], writes=[vn])
                po = nextP()
                for h in range(4):
                    k.op("pe", lambda e: e.matmul(po.t[cs, h * 128:(h + 1) * 128], G16.t[:, 2, h, cs], Sb.t[:, h, :], start=True, stop=False), reads=[G16, Sb], writes=[po])
                    k.op("pe", lambda e: e.matmul(po.t[cs, h * 128:(h + 1) * 128], G16.t[cs, 1, h, cs], vn.t[cs, h, :], start=False, stop=True), reads=[G16, vn], writes=[po])
                k.op("act", lambda e: e.activation(out=O.t[cs, :], in_=po.t[cs, :], func=AF.Copy), reads=[po], writes=[O])
                pS = nextP()
                for h in range(4):
                    k.op("pe", lambda e: e.matmul(pS.t[:, h * 128:(h + 1) * 128], G16.t[cs, 3, h, :], vn.t[cs, h, :], start=True, stop=True), reads=[G16, vn], writes=[pS])
                for h in range(4):
                    k.op("dve", lambda e: e.scalar_tensor_tensor(out=Sf.t[:, h, :], in0=Sf.t[:, h, :], scalar=G32.t[:, 512 + c * 4 + h:512 + c * 4 + h + 1],
                                                                 in1=pS.t[:, h * 128:(h + 1) * 128], op0=ALU.mult, op1=ALU.add), reads=[G32, pS], writes=[Sf])
                k.op("act", lambda e: e.activation(out=Sb.t[:], in_=Sf.t[:], func=AF.Copy), reads=[Sf], writes=[Sb])
            k.dma("sp", S["ODN"][b, d, ti * 128:(ti + 1) * 128, :], O.t[:], O, reads=[O], writes=[k.dk(("ODN", b, d, ti))])


Prog.phase_gdn_b = phase_gdn_b


def bcast_load(self, name, src_row_ap, n):
    k = self.k
    t = k.sb(name, [128, n], F32)
    k.dma("sp", t.t[:], src_row_ap.broadcast_to([128, n]), t, reads=[k.dk(("GROW", 0)), k.dk(("GROW", 1)), k.dk(("GROW", 2)), k.dk(("GROW", 3))], writes=[t])
    return t


def resid_ln(self, xt, ysrc, ybufs, GB, LNG, LNB, r, small, outb):
    k, cfg = self.k, self.cfg
    for half in range(2):
        hs = slice(half * 512, (half + 1) * 512)
        k.op("dve", lambda e: e.tensor_tensor(out=r.t[:, hs], in0=ysrc[half], in1=GB.t[:, hs], op=ALU.mult), reads=[ybufs[half], GB], writes=[r])
    k.op("dve", lambda e: e.scalar_tensor_tensor(out=r.t[:], in0=xt.t[:], scalar=float(cfg.ALPHA), in1=r.t[:], op0=ALU.mult, op1=ALU.add), reads=[xt, r], writes=[r])
    st = small.t[:, 0:12].rearrange("p (a s) -> p a s", a=2)
    for half in range(2):
        k.op("dve", lambda e: e.bn_stats(out=st[:, half, :], in_=r.t[:, half * 512:(half + 1) * 512]), reads=[r], writes=[small])
    k.op("dve", lambda e: e.bn_aggr(out=small.t[:, 12:14], in_=small.t[:, 0:12]), reads=[small], writes=[small])
    k.op("dve", lambda e: e.tensor_scalar(out=small.t[:, 14:15], in0=small.t[:, 13:14], scalar1=1e-5, scalar2=None, op0=ALU.add), reads=[small], writes=[small])
    k.op("act", lambda e: e.activation(out=small.t[:, 14:15], in_=small.t[:, 14:15], func=AF.Sqrt), reads=[small], writes=[small])
    k.op("dve", lambda e: e.reciprocal(out=small.t[:, 15:16], in_=small.t[:, 14:15]), reads=[small], writes=[small])
    k.op("dve", lambda e: e.tensor_scalar(out=r.t[:], in0=r.t[:], scalar1=small.t[:, 12:13], scalar2=small.t[:, 15:16], op0=ALU.subtract, op1=ALU.mult), reads=[r, small], writes=[r])
    k.op("pool", lambda e: e.tensor_tensor(out=r.t[:], in0=r.t[:], in1=LNG.t[:], op=ALU.mult), reads=[r, LNG], writes=[r])
    k.op("pool", lambda e: e.tensor_tensor(out=outb.t[:], in0=r.t[:], in1=LNB.t[:], op=ALU.add), reads=[r, LNB], writes=[outb])


def phase_merge(self, l):
    cfg, k, I, S = self.cfg, self.k, self.I, self.S
    NB, CTX, NTC, NT = cfg.NB, cfg.CTX, cfg.NTC, cfg.NT
    P = self.P
    pc = {"i": 0}

    def nextP():
        pc["i"] += 1
        return P[pc["i"] % 8]

    NAG = bcast_load(self, "nag", I["na_out_g"][l:l + 1, :], 512)
    DNG = bcast_load(self, "dng", I["dn_norm_g"][l:l + 1, :], 128)
    G1 = [bcast_load(self, f"g1b{v}", S["GROW"][l, 0, v:v + 1, :], 1024) for v in range(NB + 1)]
    LNG = bcast_load(self, "ln1g", I["ln1_g"][l:l + 1, :], 1024)
    LNB = bcast_load(self, "ln1b", I["ln1_b"][l:l + 1, :], 1024)
    wout = k.sb("wout", [128, 8, 1024], BF16)
    k.dma("pool", wout.t[:], I["w_out"][l].rearrange("(kc p) n -> p kc n", p=128), wout, writes=[wout])
    NS = 2
    ona = [k.sb(f"m_ona{i}", [128, 512], F32) for i in range(NS)]
    odf = [k.sb(f"m_odf{i}", [128, 512], F32) for i in range(NS)]
    odb = [k.sb(f"m_odb{i}", [128, 512], F32) for i in range(NS)]
    zs = [k.sb(f"m_zs{i}", [128, 512], BF16) for i in range(NS)]
    xt = [k.sb(f"m_x{i}", [128, 1024], F32) for i in range(NS)]
    sq = k.sb("m_sq", [128, 512], F32)
    tmp = k.sb("m_tmp", [128, 512], F32)
    st = [k.sb(f"m_st{i}", [128, 8], F32) for i in range(NS)]
    mg = [k.sb(f"m_mg{i}", [128, 1024], BF16) for i in range(NS)]
    mT = [k.sb(f"m_mT{i}", [128, 8, 128], BF16) for i in range(NS)]
    r = [k.sb(f"m_r{i}", [128, 1024], F32) for i in range(NS)]
    sm = [k.sb(f"m_sm{i}", [128, 16], F32) for i in range(NS)]
    ob = [k.sb(f"m_ob{i}", [128, 1024], F32) for i in range(NS)]
    src = I["x0"] if l == 0 else S["X"]
    it = 0
    for b in range(NB):
        for ti in range(NT):
            u = it % NS
            it += 1
            v = NB if ti < NTC else b
            rows = slice(ti * 128, (ti + 1) * 128)
            ona_keys = [k.dk(("ONA", b, h, R)) for h in range(8) for R in range(-1, cfg.ROWS // 8)]
            k.dma("sp", ona[u].t[:], S["ONA"][b, rows, :], ona[u], reads=ona_keys, writes=[ona[u]])
            k.dma("sp", odf[u].t[:], S["ODN"][b, 0, rows, :], odf[u], reads=[k.dk(("ODN", b, 0, ti))], writes=[odf[u]])
            k.dma("sp", odb[u].t[:], S["ODN"][b, 1, rows, :], odb[u], reads=[k.dk(("ODN", b, 1, ti))], writes=[odb[u]])
            k.dma("sp", zs[u].t[:], S["ZS"][b, rows, :], zs[u], reads=[k.dk(("ZS", b, ti * 128))], writes=[zs[u]])
            k.dma("sp", xt[u].t[:], src[b, rows, :], xt[u], reads=[k.dk(("X", b, ti))], writes=[xt[u]])
            O, F, B_, Z, ST = ona[u], odf[u], odb[u], zs[u], st[u]
            k.op("pool", lambda e: e.tensor_tensor(out=F.t[:], in0=F.t[:], in1=B_.t[:], op=ALU.add), reads=[F, B_], writes=[F])
            k.op("act", lambda e: e.activation(out=sq.t[:], in_=O.t[:], func=AF.Square, accum_out=ST.t[:, 0:1]), reads=[O], writes=[sq, ST])
            k.op("dve", lambda e: e.tensor_tensor(out=tmp.t[:], in0=F.t[:], in1=F.t[:], op=ALU.mult), reads=[F], writes=[tmp])
            k.op("dve", lambda e: e.tensor_reduce(out=ST.t[:, 1:5], in_=tmp.t[:].rearrange("p (h x) -> p h x", x=128), axis=AX.X, op=ALU.add), reads=[tmp], writes=[ST])
            k.op("dve", lambda e: e.tensor_scalar(out=ST.t[:, 0:1], in0=ST.t[:, 0:1], scalar1=1.0 / 512, scalar2=1e-6, op0=ALU.mult, op1=ALU.add), reads=[ST], writes=[ST])
            k.op("dve", lambda e: e.tensor_scalar(out=ST.t[:, 1:5], in0=ST.t[:, 1:5], scalar1=1.0 / 128, scalar2=1e-6, op0=ALU.mult, op1=ALU.add), reads=[ST], writes=[ST])
            k.op("act", lambda e: e.activation(out=ST.t[:, 0:5], in_=ST.t[:, 0:5], func=AF.Sqrt), reads=[ST], writes=[ST])
            k.op("dve", lambda e: e.reciprocal(out=ST.t[:, 0:5], in_=ST.t[:, 0:5]), reads=[ST], writes=[ST])
            M = mg[u]
            k.op("dve", lambda e: e.scalar_tensor_tensor(out=M.t[:, 0:512], in0=O.t[:], scalar=ST.t[:, 0:1], in1=NAG.t[:], op0=ALU.mult, op1=ALU.mult), reads=[O, ST, NAG], writes=[M])
            k.op("dve", lambda e: e.tensor_tensor(out=tmp.t[:].rearrange("p (h x) -> p h x", x=128), in0=F.t[:].rearrange("p (h x) -> p h x", x=128),
                                                  in1=ST.t[:, 1:5].unsqueeze(2).broadcast_to([128, 4, 128]), op=ALU.mult), reads=[F, ST], writes=[tmp])
            k.op("pool", lambda e: e.tensor_tensor(out=tmp.t[:].rearrange("p (h x) -> p h x", x=128), in0=tmp.t[:].rearrange("p (h x) -> p h x", x=128),
                                                   in1=DNG.t[:].unsqueeze(1).broadcast_to([128, 4, 128]), op=ALU.mult), reads=[tmp, DNG], writes=[tmp])
            k.op("dve", lambda e: e.tensor_tensor(out=M.t[:, 512:1024], in0=tmp.t[:], in1=Z.t[:], op=ALU.mult), reads=[tmp, Z], writes=[M])
            pt = nextP()
            ptb = pt.t[:].bitcast(BF16)
            for kc in range(8):
                k.op("pe", lambda e: e.transpose(out=ptb[:, kc * 128:(kc + 1) * 128], in_=M.t[:, kc * 128:(kc + 1) * 128], identity=self.identB.t[:]), reads=[M, self.identB], writes=[pt])
            k.op("act", lambda e: e.activation(out=mT[u].t[:], in_=ptb.rearrange("p (c x) -> p c x", x=128), func=AF.Copy), reads=[pt], writes=[mT[u]])
            py = [nextP(), nextP()]
            for half in range(2):
                for kc in range(8):
                    k.op("pe", lambda e: e.matmul(py[half].t[:, :], mT[u].t[:, kc, :], wout.t[:, kc, half * 512:(half + 1) * 512], start=(kc == 0), stop=(kc == 7)),
                         reads=[mT[u], wout], writes=[py[half]])
            resid_ln(self, xt[u], [py[0].t[:, :], py[1].t[:, :]], py, G1[v], LNG, LNB, r[u], sm[u], ob[u])
            k.dma("sp", S["X"][b, rows, :], ob[u].t[:], ob[u], reads=[ob[u]], writes=[k.dk(("X", b, ti))])


Prog.phase_merge = phase_merge


def phase_ffn(self, l):
    cfg, k, I, S = self.cfg, self.k, self.I, self.S
    NB, CTX, NTC, NT, L = cfg.NB, cfg.CTX, cfg.NTC, cfg.NT, cfg.DEPTH
    P = self.P
    pc = {"i": 0}

    def nextP():
        pc["i"] += 1
        return P[pc["i"] % 8]

    moe = (l % 2 == 1)
    li = l // 2
    FF = cfg.FFE if moe else cfg.FFD
    NE = cfg.NEXP if moe else 1
    fbs = []
    f0 = 0
    while f0 < FF:
        n = min(512, FF - f0)
        fbs.append((f0, n))
        f0 += n
    G2 = [bcast_load(self, f"g2b{v}", S["GROW"][l, 1, v:v + 1, :], 1024) for v in range(NB + 1)]
    LNG = bcast_load(self, "ln2g", I["ln2_g"][l:l + 1, :], 1024)
    LNB = bcast_load(self, "ln2b", I["ln2_b"][l:l + 1, :], 1024)
    GT = 8
    xg = k.sb("f_xg", [128, GT, 1024], F32)
    hT = k.sb("f_hT", [128, 8, GT * 128], BF16)
    hTf = [k.sb(f"f_hTf{i}", [128, 8, 128], F32) for i in range(2)]
    ygs = [[k.sb(f"f_yg{j}_{h_}", [128, 512], F32) for h_ in range(2)] for j in range(GT)]
    wg = [k.sb(f"f_wg{i}", [128, 8, 512], BF16) for i in range(2)]
    wu = [k.sb(f"f_wu{i}", [128, 8, 512], BF16) for i in range(2)]
    wd = [k.sb(f"f_wd{i}", [128, 4, 1024], BF16) for i in range(2)]
    hid = [k.sb(f"f_hid{i}", [128, 4, GT * 128], BF16) for i in range(1)]
    sg = [k.sb(f"f_sg{i}", [128, 512], BF16) for i in range(2)]
    r = [k.sb(f"f_r{i}", [128, 1024], F32) for i in range(1)]
    sm = [k.sb(f"f_sm{i}", [128, 16], F32) for i in range(2)]
    ob = [k.sb(f"f_ob{i}", [128, 1024], F32) for i in range(2)]
    if moe:
        rw = k.sb("f_rw", [128, 8, 8], F32)
        k.dma("sp", rw.t[:], I["moe_router"][li].rearrange("(kc p) e -> p kc e", p=128), rw, writes=[rw])
        gates = k.sb("f_gates", [128, GT, 8], F32)
        gs = [k.sb(f"f_gs{i}", [128, 4, 8], F32) for i in range(2)]
    tiles = [(b, ti) for b in range(NB) for ti in range(NT)]
    cnt = {"w": 0, "s": 0, "t": 0, "o": 0}
    last = (l == L - 1)
    for g0 in range(0, len(tiles), GT):
        gt = tiles[g0:g0 + GT]
        ng = len(gt)
        for j in range(ng):
            for h_ in range(2):
                k.op("pool", lambda e: e.memset(ygs[j][h_].t[:], 0.0), writes=[ygs[j][h_]])
        for j, (b, ti) in enumerate(gt):
            v = NB if ti < NTC else b
            k.dma("sp", xg.t[:, j, :], S["X"][b, ti * 128:(ti + 1) * 128, :], xg, reads=[k.dk(("X", b, ti))], writes=[xg])
            hf = hTf[j % 2]
            for q in range(2):
                pb = nextP()
                for c4 in range(4):
                    kc = q * 4 + c4
                    k.op("pe", lambda e: e.transpose(out=pb.t[:, c4 * 128:(c4 + 1) * 128], in_=xg.t[:, j, kc * 128:(kc + 1) * 128], identity=self.identF.t[:]), reads=[xg, self.identF], writes=[pb])
                for c4 in range(4):
                    kc = q * 4 + c4
                    k.op("act", lambda e: e.activation(out=hf.t[:, kc, :], in_=pb.t[:, c4 * 128:(c4 + 1) * 128], func=AF.Identity,
                                                       scale=self.modT.t[:, l, 32 + kc, v:v + 1], bias=self.modT.t[:, l, 24 + kc, v:v + 1]), reads=[pb, self.modT], writes=[hf])
            k.op("dve", lambda e: e.tensor_copy(out=hT.t[:, :, j * 128:(j + 1) * 128], in_=hf.t[:]), reads=[hf], writes=[hT])
            if moe:
                pl = nextP()
                for kc in range(8):
                    k.op("pe", lambda e: e.matmul(pl.t[:, 0:8], hf.t[:, kc, :], rw.t[:, kc, :], start=(kc == 0), stop=(kc == 7)), reads=[hf, rw], writes=[pl])
                GS = gs[j % 2]
                lg, t8, mk_, ex_ = GS.t[:, 0, :], GS.t[:, 1, :], GS.t[:, 2, :], GS.t[:, 3, :]
                k.op("dve", lambda e: e.tensor_copy(out=lg, in_=pl.t[:, 0:8]), reads=[pl], writes=[GS])
                k.op("dve", lambda e: e.max(out=t8, in_=lg), reads=[GS], writes=[GS])
                k.op("dve", lambda e: e.tensor_scalar(out=mk_, in0=lg, scalar1=GS.t[:, 1, 1:2], scalar2=None, op0=ALU.is_ge), reads=[GS], writes=[GS])
                k.op("dve", lambda e: e.tensor_scalar(out=ex_, in0=lg, scalar1=GS.t[:, 1, 0:1], scalar2=None, op0=ALU.subtract), reads=[GS], writes=[GS])
                k.op("act", lambda e: e.activation(out=ex_, in_=ex_, func=AF.Exp), reads=[GS], writes=[GS])
                k.op("dve", lambda e: e.tensor_tensor(out=ex_, in0=ex_, in1=mk_, op=ALU.mult), reads=[GS], writes=[GS])
                k.op("dve", lambda e: e.tensor_reduce(out=GS.t[:, 1, 2:3], in_=ex_, axis=AX.X, op=ALU.add), reads=[GS], writes=[GS])
                k.op("dve", lambda e: e.reciprocal(out=GS.t[:, 1, 3:4], in_=GS.t[:, 1, 2:3]), reads=[GS], writes=[GS])
                k.op("dve", lambda e: e.tensor_scalar(out=gates.t[:, j, :], in0=ex_, scalar1=GS.t[:, 1, 3:4], scalar2=None, op0=ALU.mult), reads=[GS], writes=[gates])
        ntok = ng * 128
        sbs = [(s0, min(512, ntok - s0)) for s0 in range(0, ntok, 512)]
        work = [(e_, f0, fn) for e_ in range(NE) for (f0, fn) in fbs]

        def load(idx):
            e_, f0, fn = work[idx]
            wi = idx % 2
            if moe:
                Wg, Wu, Wd = I["moe_w_gate"][li, e_], I["moe_w_up"][li, e_], I["moe_w_down"][li, e_]
            else:
                Wg, Wu, Wd = I["ffn_w_gate"][li], I["ffn_w_up"][li], I["ffn_w_down"][li]
            nfc = fn // 128
            k.dma("pool", wg[wi].t[:, :, 0:fn], Wg[:, f0:f0 + fn].rearrange("(kc p) n -> p kc n", p=128), wg[wi], writes=[wg[wi]])
            k.dma("pool", wu[wi].t[:, :, 0:fn], Wu[:, f0:f0 + fn].rearrange("(kc p) n -> p kc n", p=128), wu[wi], writes=[wu[wi]])
            k.dma("pool", wd[wi].t[:, 0:nfc, :], Wd[f0:f0 + fn, :].rearrange("(c p) n -> p c n", p=128), wd[wi], writes=[wd[wi]])

        load(0)
        for idx, (e_, f0, fn) in enumerate(work):
            if idx + 1 < len(work):
                load(idx + 1)
            wi = idx % 2
            nfc = fn // 128
            H = hid[0]
            for (s0, sn) in sbs:
                for fc in range(nfc):
                    pg_, pu_ = nextP(), nextP()
                    for kc in range(8):
                        k.op("pe", lambda e: e.matmul(pg_.t[:, 0:sn], wg[wi].t[:, kc, fc * 128:(fc + 1) * 128], hT.t[:, kc, s0:s0 + sn], start=(kc == 0), stop=(kc == 7)), reads=[wg[wi], hT], writes=[pg_])
                    for kc in range(8):
                        k.op("pe", lambda e: e.matmul(pu_.t[:, 0:sn], wu[wi].t[:, kc, fc * 128:(fc + 1) * 128], hT.t[:, kc, s0:s0 + sn], start=(kc == 0), stop=(kc == 7)), reads=[wu[wi], hT], writes=[pu_])
                    SG = sg[cnt["s"] % 2]
                    cnt["s"] += 1
                    k.op("act", lambda e: e.activation(out=SG.t[:, 0:sn], in_=pg_.t[:, 0:sn], func=AF.Silu), reads=[pg_], writes=[SG])
                    k.op("dve", lambda e: e.tensor_tensor(out=H.t[:, fc, s0:s0 + sn], in0=pu_.t[:, 0:sn], in1=SG.t[:, 0:sn], op=ALU.mult), reads=[pu_, SG], writes=[H])
            for j in range(ng):
                for half in range(2):
                    pd_ = nextP()
                    for fc in range(nfc):
                        k.op("pe", lambda e: e.matmul(pd_.t[:, :], H.t[:, fc, j * 128:(j + 1) * 128], wd[wi].t[:, fc, half * 512:(half + 1) * 512], start=(fc == 0), stop=(fc == nfc - 1)),
                             reads=[H, wd[wi]], writes=[pd_])
                    Y = ygs[j][half]
                    if moe:
                        k.op("dve", lambda e: e.scalar_tensor_tensor(out=Y.t[:], in0=pd_.t[:, :], scalar=gates.t[:, j, e_:e_ + 1], in1=Y.t[:], op0=ALU.mult, op1=ALU.add),
                             reads=[pd_, gates, Y], writes=[Y])
                    else:
                        k.op("dve", lambda e: e.tensor_tensor(out=Y.t[:], in0=pd_.t[:, :], in1=Y.t[:], op=ALU.add), reads=[pd_, Y], writes=[Y])
        for j, (b, ti) in enumerate(gt):
            v = NB if ti < NTC else b
            o_ = ob[cnt["o"] % 2]
            s_ = sm[cnt["o"] % 2]
            cnt["o"] += 1
            resid_ln_x(self, xg, j, ygs[j], G2[v], LNG, LNB, r[0], s_, o_)
            if last:
                if ti >= NTC:
                    k.dma("sp", self.out[b, (ti - NTC) * 128:(ti - NTC + 1) * 128, :], o_.t[:], o_, reads=[o_], writes=[k.dk(("Y", b, ti))])
            else:
                k.dma("sp", S["X"][b, ti * 128:(ti + 1) * 128, :], o_.t[:], o_, reads=[o_], writes=[k.dk(("X", b, ti))])


def resid_ln_x(self, xg, j, yg, GB, LNG, LNB, r, small, outb):
    class V:
        pass
    xv = V()
    xv.t = xg.t[:, j, :]
    class A:
        def __init__(s, ap):
            s.ap = ap
        def __getitem__(s, idx):
            return s.ap
    xa = Buf(A(xg.t[:, j, :]), "xa")
    xa.w, xa.r = xg.w, xg.r
    resid_ln(self, xa, [yg[0].t[:], yg[1].t[:]], [yg[0], yg[1]], GB, LNG, LNB, r, small, outb)
    for s_, v_ in xa.r.items():
        if xg.r.get(s_, 0) < v_:
            xg.r[s_] = v_


Prog.phase_ffn = phase_ffn


_PROG = {}


def kernel(**inputs):
    cfg = Cfg()
    inp = {k_: np.asarray(v) for k_, v in inputs.items()}
    if "prog" not in _PROG:
        _PROG["prog"] = Prog(cfg)
    prog = _PROG["prog"]
    shared = shared_inputs(cfg, inp)
    names = set(prog.I.keys())
    in_maps = []
    for core in range(cfg.NCORES):
        ci = prep_core_inputs(cfg, inp, core)
        ci.update(shared)
        in_maps.append({k_: v for k_, v in ci.items() if k_ in names})
    res = run_bass_kernel_spmd(prog.k.nc, in_maps, core_ids=list(range(cfg.NCORES)))
    out = np.concatenate([r["y"] for r in res.results], axis=0)
    return out.astype(np.float32)
```
